# Optimizing a Trainium2 kernel written in Bass

```python
import jax
import jax.numpy as jnp
from jax import lax
import numpy as np

D_MODEL = 1024
BATCH = 16
SEQ = 4096
DEPTH = 1
DEC_BATCH = 8
DEC_SEQ = 32
PAST_LEN = 2048

CHUNK = 64
HG_HEADS = 4
HG_DK = 128
HG_DV = 128
HG_QK_WIDTH = HG_HEADS * HG_DK
HG_WIDTH = HG_HEADS * HG_DV
ML_HEADS = 4
ML_DK = 128
ML_DV = 128
ML_QK_WIDTH = ML_HEADS * ML_DK
ML_WIDTH = ML_HEADS * ML_DV
XA_HEADS = 4
XA_DH = 128
XA_WIDTH = XA_HEADS * XA_DH
N_MEM = 256
N_BRANCH = 3
BRANCH_WIDTH = 512
D_FF = 2816
CONV_W = 3
NORM_EPS = 1e-6
IN_SPLITS = (HG_QK_WIDTH, HG_QK_WIDTH, HG_WIDTH, HG_WIDTH,
             ML_QK_WIDTH, ML_QK_WIDTH, ML_WIDTH, ML_WIDTH, ML_HEADS, ML_HEADS,
             XA_WIDTH, N_BRANCH * D_MODEL)
N_IN = (2 * HG_QK_WIDTH + 2 * HG_WIDTH + 2 * ML_QK_WIDTH + 2 * ML_WIDTH
        + 2 * ML_HEADS + XA_WIDTH + N_BRANCH * D_MODEL)

kernel_name = 'hgrn2_mlstm_memxattn_convffn_stream_step'


def rms_norm(x, g):
    xf = x.astype(jnp.float32)
    y = xf * lax.rsqrt(jnp.mean(xf * xf, axis=-1, keepdims=True) + NORM_EPS)
    return (y * g.astype(jnp.float32)).astype(x.dtype)


def split_heads(x, n_heads):
    return x.reshape(x.shape[:-1] + (n_heads, x.shape[-1] // n_heads))


def block_len(T):
    return CHUNK if T % CHUNK == 0 else T


def to_blocks(a, L):
    B, T = a.shape[:2]
    a = a.astype(jnp.float32).reshape((B, T // L, L) + a.shape[2:])
    return jnp.moveaxis(jnp.moveaxis(a, 1, 0), 2, 3)


def from_blocks(a):
    NC, B, H, L, d = a.shape
    return jnp.transpose(a, (1, 0, 3, 2, 4)).reshape(B, NC * L, H, d)


def hgrn2_recurrence(q, log_f, k, v, S0):
    L = block_len(q.shape[1])
    qb, gb, kb, vb = (to_blocks(a, L) for a in (q, log_f, k, v))
    causal = jnp.tril(jnp.ones((L, L), dtype=bool))

    def step(S, blk):
        qc, gc, kc, vc = blk
        A = jnp.cumsum(gc, axis=2)
        rel = jnp.where(causal[:, :, None], A[:, :, :, None, :] - A[:, :, None, :, :], -jnp.inf)
        scores = jnp.einsum('bhtc,bhsc,bhtsc->bhts', qc, kc, jnp.exp(rel))
        o = (jnp.einsum('bhts,bhsv->bhtv', scores, vc)
             + jnp.einsum('bhtc,bhcv->bhtv', qc * jnp.exp(A), S))
        A_last = A[:, :, -1:, :]
        S_new = (jnp.exp(A_last[:, :, 0, :, None]) * S
                 + jnp.einsum('bhsc,bhsv->bhcv', kc * jnp.exp(A_last - A), vc))
        return S_new, o

    S_fin, ob = lax.scan(step, S0.astype(jnp.float32), (qb, gb, kb, vb))
    return from_blocks(ob), S_fin


def mlstm_recurrence(q, k, v, i_pre, log_f, C0, n0, m0):
    L = block_len(q.shape[1])
    qb, kb, vb = (to_blocks(a, L) for a in (q, k, v))
    ib, fb = (to_blocks(a, L) for a in (i_pre, log_f))
    causal = jnp.tril(jnp.ones((L, L), dtype=bool))

    def step(carry, blk):
        C, n, m = carry
        qc, kc, vc, ic, fc = blk
        b = jnp.cumsum(fc, axis=-1)
        log_w = jnp.where(causal, b[..., :, None] - b[..., None, :] + ic[..., None, :], -jnp.inf)
        log_inter = b + m[..., None]
        m_t = jnp.maximum(log_inter, jnp.max(log_w, axis=-1))
        w = jnp.exp(log_w - m_t[..., None])
        a = jnp.exp(log_inter - m_t)
        s = jnp.einsum('bhtd,bhsd->bhts', qc, kc) * w
        num = (jnp.einsum('bhts,bhsv->bhtv', s, vc)
               + a[..., None] * jnp.einsum('bhtd,bhdv->bhtv', qc, C))
        den = jnp.sum(s, axis=-1) + a * jnp.einsum('bhtd,bhd->bht', qc, n)
        h = num / jnp.maximum(jnp.abs(den), jnp.exp(-m_t))[..., None]
        log_last = b[..., -1:] - b + ic
        m_new = jnp.maximum(b[..., -1] + m, jnp.max(log_last, axis=-1))
        wk = jnp.exp(log_last - m_new[..., None])
        decay = jnp.exp(b[..., -1] + m - m_new)
        C_new = decay[..., None, None] * C + jnp.einsum('bhs,bhsd,bhsv->bhdv', wk, kc, vc)
        n_new = decay[..., None] * n + jnp.einsum('bhs,bhsd->bhd', wk, kc)
        return (C_new, n_new, m_new), h

    init = (C0.astype(jnp.float32), n0.astype(jnp.float32), m0.astype(jnp.float32))
    (C, n, m), hb = lax.scan(step, init, (qb, kb, vb, ib, fb))
    return from_blocks(hb), C, n, m


def memory_kv(mem, g, w_mem_kv):
    B = mem.shape[0]
    k, v = jnp.split(rms_norm(mem, g) @ w_mem_kv, 2, axis=-1)
    return (k.reshape(B, N_MEM, XA_HEADS, XA_DH), v.reshape(B, N_MEM, XA_HEADS, XA_DH))


def encoder_layer(h, s_hgrn, s_C, s_n, s_m, s_conv, mem_k, mem_v, lb,
                  norm1, w_in, b_in, ml_fgate_bias, hg_norm, ml_norm, w_branch, w_out,
                  norm2, w_up, ffn_conv_w, ffn_conv_b, w_down):
    B, T, _ = h.shape
    dt = h.dtype
    split_points = np.cumsum(IN_SPLITS)[:-1].tolist()
    u = rms_norm(h, norm1)
    z = u @ w_in + b_in
    hq, hf, hi, hg, mq, mk, mv, mo, mi, mf, xq, gl = jnp.split(z, split_points, axis=-1)

    f = lb + (1.0 - lb) * jax.nn.sigmoid(hf.astype(jnp.float32))
    o_h, s_hgrn = hgrn2_recurrence(split_heads(jax.nn.silu(hq), HG_HEADS),
                                   split_heads(jnp.log(f), HG_HEADS),
                                   split_heads(1.0 - f, HG_HEADS),
                                   split_heads(hi, HG_HEADS), s_hgrn)
    o_h = rms_norm(o_h.astype(dt), hg_norm.reshape(HG_HEADS, HG_DV)).reshape(B, T, HG_WIDTH)
    o_h = o_h * jax.nn.silu(hg)

    log_fg = jax.nn.log_sigmoid((mf + ml_fgate_bias).astype(jnp.float32))
    h_m, s_C, s_n, s_m = mlstm_recurrence(split_heads(mq, ML_HEADS),
                                          split_heads(mk, ML_HEADS) * (ML_DK ** -0.5),
                                          split_heads(mv, ML_HEADS),
                                          mi.astype(jnp.float32), log_fg, s_C, s_n, s_m)
    h_m = rms_norm(h_m.astype(dt), ml_norm.reshape(ML_HEADS, ML_DV)).reshape(B, T, ML_WIDTH)
    h_m = h_m * jax.nn.sigmoid(mo)

    q_x = split_heads(xq, XA_HEADS)
    logits = jnp.einsum('bthd,bmhd->bhtm', q_x, mem_k) * (XA_DH ** -0.5)
    p = jax.nn.softmax(logits.astype(jnp.float32), axis=-1).astype(dt)
    o_x = jnp.einsum('bhtm,bmhd->bthd', p, mem_v).reshape(B, T, XA_WIDTH)

    gates = jax.nn.sigmoid(gl.reshape(B, T, N_BRANCH, D_MODEL))
    merged = gates[:, :, 0] * (o_h @ w_branch[0])
    merged = merged + gates[:, :, 1] * (h_m @ w_branch[1])
    merged = merged + gates[:, :, 2] * (o_x @ w_branch[2])
    h = h + merged @ w_out

    up_g, up_v = jnp.split(rms_norm(h, norm2) @ w_up, 2, axis=-1)
    full = jnp.concatenate([s_conv.astype(up_g.dtype), up_g], axis=1)
    conv = ffn_conv_b + ffn_conv_w[0] * full[:, 0:T]
    for j in range(1, CONV_W):
        conv = conv + ffn_conv_w[j] * full[:, j:j + T]
    s_conv = full[:, T:]
    h = h + (jax.nn.gelu(conv) * up_v) @ w_down
    return (h, s_hgrn.astype(dt), s_C.astype(dt), s_n.astype(dt), s_m.astype(dt), s_conv)


def setup_inputs(seed: int = 0) -> dict:
    key = jax.random.key(seed)
    ks = jax.random.split(key, 32)
    f32 = jnp.float32

    def nrm(k, shape, scale):
        return scale * jax.random.normal(k, shape, f32)

    def gain(k, shape):
        return 1.0 + 0.05 * jax.random.normal(k, shape, f32)

    return {
        'x_prompt': nrm(ks[0], (BATCH, SEQ, D_MODEL), 1.0),
        'x_sample': nrm(ks[1], (DEC_BATCH, DEC_SEQ, D_MODEL), 1.0),
        'state_hgrn': nrm(ks[2], (DEPTH, DEC_BATCH, HG_HEADS, HG_DK, HG_DV), 0.5),
        'state_mlstm_C': nrm(ks[3], (DEPTH, DEC_BATCH, ML_HEADS, ML_DK, ML_DV), 0.1),
        'state_mlstm_n': nrm(ks[4], (DEPTH, DEC_BATCH, ML_HEADS, ML_DK), 0.1),
        'state_mlstm_m': nrm(ks[5], (DEPTH, DEC_BATCH, ML_HEADS), 1.0),
        'state_ffn_conv': nrm(ks[6], (DEPTH, DEC_BATCH, CONV_W - 1, D_FF), 1.0),
        'cache_mem_k': nrm(ks[7], (DEPTH, DEC_BATCH, N_MEM, XA_HEADS, XA_DH), 1.0),
        'cache_mem_v': nrm(ks[8], (DEPTH, DEC_BATCH, N_MEM, XA_HEADS, XA_DH), 1.0),
        'mem_prompt': nrm(ks[9], (BATCH, N_MEM, D_MODEL), 1.0),
        'norm1': gain(ks[10], (DEPTH, D_MODEL)),
        'w_in': nrm(ks[11], (DEPTH, D_MODEL, N_IN), D_MODEL ** -0.5),
        'b_in': nrm(ks[12], (DEPTH, N_IN), 0.02),
        'ml_fgate_bias': jnp.linspace(3.0, 6.0, ML_HEADS, dtype=f32)[None, :] + nrm(ks[13], (DEPTH, ML_HEADS), 0.1),
        'hg_lb_logits': nrm(ks[14], (DEPTH + 1, HG_QK_WIDTH), 0.5),
        'hg_norm': gain(ks[15], (DEPTH, HG_WIDTH)),
        'ml_norm': gain(ks[16], (DEPTH, ML_WIDTH)),
        'mem_norm': gain(ks[17], (DEPTH, D_MODEL)),
        'w_mem_kv': nrm(ks[18], (DEPTH, D_MODEL, 2 * XA_WIDTH), D_MODEL ** -0.5),
        'w_branch': nrm(ks[19], (DEPTH, N_BRANCH, BRANCH_WIDTH, D_MODEL), BRANCH_WIDTH ** -0.5),
        'w_out': nrm(ks[20], (DEPTH, D_MODEL, D_MODEL), D_MODEL ** -0.5),
        'norm2': gain(ks[21], (DEPTH, D_MODEL)),
        'w_up': nrm(ks[22], (DEPTH, D_MODEL, 2 * D_FF), D_MODEL ** -0.5),
        'ffn_conv_w': nrm(ks[23], (DEPTH, CONV_W, D_FF), CONV_W ** -0.5),
        'ffn_conv_b': nrm(ks[24], (DEPTH, D_FF), 0.02),
        'w_down': nrm(ks[25], (DEPTH, D_FF, D_MODEL), D_FF ** -0.5),
        'final_norm': gain(ks[26], (D_MODEL,)),
    }


def reference(x_prompt, x_sample, state_hgrn, state_mlstm_C, state_mlstm_n, state_mlstm_m,
              state_ffn_conv, cache_mem_k, cache_mem_v, mem_prompt,
              norm1, w_in, b_in, ml_fgate_bias, hg_lb_logits, hg_norm, ml_norm, mem_norm,
              w_mem_kv, w_branch, w_out, norm2, w_up, ffn_conv_w, ffn_conv_b, w_down, final_norm):
    dt = x_prompt.dtype
    Bp = x_prompt.shape[0]
    lbs = jnp.cumsum(jax.nn.softmax(hg_lb_logits.astype(jnp.float32), axis=0), axis=0)
    hp, hs = x_prompt, x_sample
    p_hg, p_C, p_n, p_m, p_mk, p_mv, p_cv = [], [], [], [], [], [], []
    s_hg, s_C, s_n, s_m, s_cv = [], [], [], [], []
    for l in range(DEPTH):
        lw = (norm1[l], w_in[l], b_in[l], ml_fgate_bias[l], hg_norm[l], ml_norm[l], w_branch[l],
              w_out[l], norm2[l], w_up[l], ffn_conv_w[l], ffn_conv_b[l], w_down[l])
        mk_p, mv_p = memory_kv(mem_prompt, mem_norm[l], w_mem_kv[l])
        hp, a1, a2, a3, a4, a5 = encoder_layer(
            hp,
            jnp.zeros((Bp, HG_HEADS, HG_DK, HG_DV), dt),
            jnp.zeros((Bp, ML_HEADS, ML_DK, ML_DV), dt),
            jnp.zeros((Bp, ML_HEADS, ML_DK), dt),
            jnp.zeros((Bp, ML_HEADS), dt),
            jnp.zeros((Bp, CONV_W - 1, D_FF), dt),
            mk_p, mv_p, lbs[l], *lw)
        p_hg.append(a1); p_C.append(a2); p_n.append(a3); p_m.append(a4); p_cv.append(a5)
        p_mk.append(mk_p); p_mv.append(mv_p)
        hs, b1, b2, b3, b4, b5 = encoder_layer(
            hs, state_hgrn[l], state_mlstm_C[l], state_mlstm_n[l], state_mlstm_m[l],
            state_ffn_conv[l], cache_mem_k[l], cache_mem_v[l], lbs[l], *lw)
        s_hg.append(b1); s_C.append(b2); s_n.append(b3); s_m.append(b4); s_cv.append(b5)
    y_prompt = rms_norm(hp, final_norm)
    y_sample = rms_norm(hs, final_norm)
    return (y_prompt, y_sample,
            jnp.stack(p_hg), jnp.stack(p_C), jnp.stack(p_n), jnp.stack(p_m),
            jnp.stack(p_mk), jnp.stack(p_mv), jnp.stack(p_cv),
            jnp.stack(s_hg), jnp.stack(s_C), jnp.stack(s_n), jnp.stack(s_m), jnp.stack(s_cv))
```

```python
import contextlib
import numpy as np
import concourse.bass as bass
import concourse.mybir as mybir
from concourse.bass_utils import run_bass_kernel_spmd

F32 = mybir.dt.float32
BF16 = mybir.dt.bfloat16
AF = mybir.ActivationFunctionType
ALU = mybir.AluOpType
AX = mybir.AxisListType

ENGS = ("pe", "act", "dve", "pool", "sp")

D = 1024
NIN = 7688
DFF = 2816
NFC = 22
EPS = 1e-6
NMEM = 256
C_HQ, C_HF, C_HI, C_HG, C_MQ, C_MK, C_MV, C_MO, C_MI, C_MF, C_XQ, C_GL = (
    0, 512, 1024, 1536, 2048, 2560, 3072, 3584, 4096, 4100, 4104, 4616)


class Sched:
    LAT = 150.0

    def __init__(self, nc):
        self.nc = nc
        self.recs = []
        self.lastw = {}
        self.readers = {}
        self.last_dma = {}
        self.dma_cnt = {}
        self.final_waits = {}
        self.cnt = {e: 0 for e in ENGS}
        self.reorder = True
        self.trace_cp = False
        self.prio = 'bl'
        self.cause = {}
        self.start = {}

    def _deps(self, eng, reads, writes):
        deps = set()
        for k in reads:
            w = self.lastw.get(k)
            if w is not None:
                deps.add(w)
            if k.startswith("ps"):
                for t in self.readers.get(k, ()):
                    if self.recs[t]["eng"] != eng:
                        deps.add(t)
        for k in writes:
            w = self.lastw.get(k)
            if w is not None:
                deps.add(w)
            for t in self.readers.get(k, ()):
                deps.add(t)
        return deps

    def _commit(self, oid, reads, writes):
        for k in reads:
            self.readers.setdefault(k, []).append(oid)
        for k in writes:
            self.lastw[k] = oid
            self.readers[k] = []

    def op(self, eng, fn, reads=(), writes=(), cost=300.0):
        oid = len(self.recs)
        self.recs.append(dict(eng=eng, fn=fn, deps=self._deps(eng, reads, writes), cost=cost, dma=None))
        self.cnt[eng] += 1
        self._commit(oid, reads, writes)
        return oid

    def dma(self, eng, fn, semkey, reads=(), writes=(), final=False, cost=4000.0):
        oid = len(self.recs)
        deps = self._deps(eng, reads, writes)
        if semkey in self.last_dma:
            deps.add(self.last_dma[semkey])
        v = self.dma_cnt.get(semkey, 0) + 16
        self.dma_cnt[semkey] = v
        self.last_dma[semkey] = oid
        self.recs.append(dict(eng=eng, fn=fn, deps=deps, cost=cost, dma=(semkey, v)))
        self.cnt[eng] += 1
        self._commit(oid, reads, writes)
        if final:
            self.final_waits[semkey] = v
        return oid

    def schedule(self):
        import heapq
        recs = self.recs
        n = len(recs)
        if not self.reorder:
            order = {e: [] for e in ENGS}
            for i, r in enumerate(recs):
                order[r["eng"]].append(i)
            return order
        succ = [[] for _ in range(n)]
        ndep = [0] * n
        for i, r in enumerate(recs):
            ndep[i] = len(r["deps"])
            for d in r["deps"]:
                succ[d].append(i)
        bl = [0.0] * n
        for i in range(n - 1, -1, -1):
            m = 0.0
            for j in succ[i]:
                v = bl[j] + self.LAT
                if v > m:
                    m = v
            bl[i] = recs[i]["cost"] + m
        ready_t = [0.0] * n
        fin = [0.0] * n
        heaps = {e: [] for e in ENGS}
        for i in range(n):
            if ndep[i] == 0:
                heapq.heappush(heaps[recs[i]["eng"]], (0.0, i))
        free = {e: 0.0 for e in ENGS}
        order = {e: [] for e in ENGS}
        done = 0
        while done < n:
            best = None
            for e in ENGS:
                h = heaps[e]
                if not h:
                    continue
                st = max(free[e], h[0][0])
                if best is None or st < best[0]:
                    best = (st, e)
            st, e = best
            h = heaps[e]
            cand = []
            while h and h[0][0] <= st:
                cand.append(heapq.heappop(h))
            pick = max(cand, key=lambda c: (bl[c[1]], -c[1])) if self.prio == 'bl' else min(cand, key=lambda c: c[1])
            for c in cand:
                if c is not pick:
                    heapq.heappush(h, c)
            i = pick[1]
            r = recs[i]
            if self.trace_cp:
                dmax, darg = -1.0, None
                for d in r["deps"]:
                    if fin[d] + self.LAT > dmax:
                        dmax, darg = fin[d] + self.LAT, d
                if order[e] and free[e] >= dmax:
                    self.cause[i] = ("eng", order[e][-1])
                else:
                    self.cause[i] = ("dep", darg)
                self.start[i] = st
            if r["dma"] is None:
                fin[i] = st + r["cost"]
                free[e] = fin[i]
            elif e == "pool":
                fin[i] = st + r["cost"]
                free[e] = st + 1200.0
            else:
                fin[i] = st + r["cost"]
                free[e] = st + 60.0
            order[e].append(i)
            done += 1
            for j in succ[i]:
                rt = fin[i] + self.LAT
                if rt > ready_t[j]:
                    ready_t[j] = rt
                ndep[j] -= 1
                if ndep[j] == 0:
                    heapq.heappush(heaps[recs[j]["eng"]], (ready_t[j], j))
        self.sim_time = max(fin) if n else 0.0
        self.fin = fin
        return order

    def emit(self, es, final_engine="sp"):
        nc = self.nc
        recs = self.recs
        order = self.schedule()
        tok = [None] * len(recs)
        for e in ENGS:
            c = 0
            for i in order[e]:
                r = recs[i]
                if r["dma"] is not None:
                    tok[i] = r["dma"]
                else:
                    c += 1
                    tok[i] = (e, c)
        sems = {}
        for e in ENGS:
            sems[e] = es.enter_context(nc.semaphore("s_" + e))
        for i, sk in enumerate(self.dma_cnt):
            sems[sk] = es.enter_context(nc.semaphore("d%d" % i))
        block = es.enter_context(nc.Block())
        fw = list(self.final_waits.items())

        def run(eng_name):
            def body(eng):
                known = {}
                for i in order[eng_name]:
                    r = recs[i]
                    need = {}
                    for d in r["deps"]:
                        sk, v = tok[d]
                        if eng_name == "pe" and sk == "pe":
                            continue
                        if need.get(sk, 0) < v:
                            need[sk] = v
                    for sk, v in need.items():
                        if known.get(sk, 0) >= v:
                            continue
                        known[sk] = v
                        eng.wait_ge(sems[sk], v)
                    ins = r["fn"](eng)
                    if r["dma"] is None:
                        ins.then_inc(sems[eng_name], 1)
                    else:
                        ins.then_inc(sems[r["dma"][0]], 16)
                if eng_name == final_engine:
                    for wk, wv in fw:
                        eng.wait_ge(sems[wk], wv)
            return body

        block.tensor(run("pe"))
        block.scalar(run("act"))
        block.vector(run("dve"))
        block.gpsimd(run("pool"))
        block.sync(run("sp"))


class Builder:
    def __init__(self, TT=256, n_prompt=2, T_prompt=4096, T_sample=32):
        self.TT = TT
        self.n_prompt = n_prompt
        self.T_prompt = T_prompt
        self.T_sample = T_sample
        self.nc = bass.Bass("TRN2", target_bir_lowering=False)
        self.es = contextlib.ExitStack()
        self.S = Sched(self.nc)
        self.uid = 0
        self.ps_free = list(range(8))
        self.ring_i = 0
        self.slot_i = 0
        self.piece_in_slot = {}

    def ck(self, name):
        import os
        if os.environ.get("STOP_AT") == name:
            raise StopIteration(name)

    def sb(self, name, shape, dt=F32):
        return self.es.enter_context(self.nc.sbuf_tensor(name, list(shape), dt))

    def dram(self, name, shape, dt=F32, kind="ExternalInput"):
        return self.nc.dram_tensor(name, list(shape), dt, kind=kind).ap()

    @staticmethod
    def _n(ap):
        n = 1
        for d in ap.shape[1:]:
            n *= int(d)
        return n

    def _ec(self, eng, ap):
        n = self._n(ap)
        if eng == "act":
            return 230.0 + 0.85 * n
        if eng == "pool":
            return 150.0 + 1.6 * n
        return 70.0 + 1.05 * n

    def act(self, out, in_, func, r, w, bias=None, scale=None, accum=None):
        kw = {}
        if bias is not None:
            kw["bias"] = bias
        if scale is not None:
            kw["scale"] = scale
        if accum is not None:
            kw["accum_out"] = accum
        self.S.op("act", lambda e: e.activation(out=out, in_=in_, func=func, **kw), r, w, cost=self._ec("act", out) + (100 if accum is not None else 0))

    def ts(self, eng, out, in0, s1, s2, op0, op1, r, w):
        c = self._ec(eng, out)
        if op1 is None:
            self.S.op(eng, lambda e: e.tensor_scalar(out=out, in0=in0, scalar1=s1, scalar2=None, op0=op0), r, w, cost=c)
        else:
            self.S.op(eng, lambda e: e.tensor_scalar(out=out, in0=in0, scalar1=s1, scalar2=s2, op0=op0, op1=op1), r, w, cost=c)

    def stt(self, out, in0, scalar, in1, op0, op1, r, w):
        self.S.op("dve", lambda e: e.scalar_tensor_tensor(out=out, in0=in0, scalar=scalar, in1=in1, op0=op0, op1=op1), r, w,
                  cost=self._ec("dve", out))

    def tt(self, eng, out, in0, in1, op, r, w):
        self.S.op(eng, lambda e: e.tensor_tensor(out=out, in0=in0, in1=in1, op=op), r, w, cost=self._ec(eng, out))

    def cp(self, eng, out, in_, r, w):
        if eng == "act":
            self.S.op("act", lambda e: e.activation(out=out, in_=in_, func=AF.Copy), r, w, cost=self._ec("act", out))
        else:
            self.S.op(eng, lambda e: e.tensor_copy(out=out, in_=in_), r, w, cost=self._ec(eng, out))

    def mm(self, out, lhsT, rhs, start, stop, r, w):
        n = self._n(rhs)
        c = 60.0 + 0.33 * max(n, 64)
        if rhs.dtype == F32:
            c *= 4
        self.S.op("pe", lambda e: e.matmul(out, lhsT=lhsT, rhs=rhs, start=start, stop=stop), r, w, cost=c)

    def tr(self, out, in_, ident, r, w):
        self.S.op("pe", lambda e: e.matmul(out, lhsT=in_, rhs=ident, start=True, stop=True, is_transpose=True), r, w, cost=170.0)

    def memset(self, eng, ap, val, r, w):
        self.S.op(eng, lambda e: e.memset(ap, val), r, w, cost=self._ec(eng, ap))

    def dma(self, out, in_, semkey, r, w, eng="sp", final=False, slow=False):
        nbytes = self._n(out) * int(out.shape[0]) * (2 if out.dtype == BF16 else 4)
        c = 2000.0 + nbytes * 0.005
        if eng == "pool":
            c = 3000.0 + nbytes * 0.018
        if slow:
            self.S.dma(eng, lambda e: e.dma_start(out=out, in_=in_, allow_slow_non_contiguous=True), semkey, r, w, final, cost=c)
        else:
            self.S.dma(eng, lambda e: e.dma_start(out=out, in_=in_), semkey, r, w, final, cost=c)

    def pow_m05(self, out, in_, r, w):
        n = in_.shape[-1]
        P = in_.shape[0]
        self.S.op("pool", lambda e: e.tensor_tensor(out=out, in0=in_, in1=self.mhalf[0:P, 0:n], op=ALU.pow), list(r) + ["consts"], w)

    def psum(self):
        if not self.ps_free:
            raise RuntimeError("out of PSUM banks")
        i = self.ps_free.pop(0)
        return self.ps[i], "ps%d" % i

    def pfree(self, pk):
        i = int(pk[2:])
        assert i not in self.ps_free
        self.ps_free.append(i)

    def load_T(self, dst, src, n, r, w, semkey):
        self.dma(self.stg[0:n, :], src, semkey, [], ["stg"])
        pt, pk = self.psum()
        self.tr(pt[:, 0:n], self.stg[0:n, :], self.ident_f[0:n, 0:n], ["stg", "consts"], [pk])
        self.cp("dve", dst, pt[:, 0:n], [pk] + list(r), w)
        self.pfree(pk)

    def store_T(self, dst, src, n, r, semkey):
        pt, pk = self.psum()
        self.tr(pt[0:n, 0:128], src, self.ident_f[:], list(r) + ["consts"], [pk])
        self.cp("dve", self.stg[0:n, :], pt[0:n, 0:128], [pk], ["stg"])
        self.pfree(pk)
        self.dma(dst, self.stg[0:n, :], semkey, ["stg"], [], final=True)

    def tmp(self):
        i = self.ring_i
        self.ring_i = (i + 1) % self.NRING
        return self.ringbuf[:, i, :], "ring%d" % i

    def tmp4(self):
        i = 0 if self.ring_i in (0, 5, 6, 7) else 4
        if self.ring_i in (1, 2, 3, 4):
            i = 4
        self.ring_i = (i + 4) % self.NRING
        return self.ringbuf[:, i:i + 4, :].rearrange("p a b -> p (a b)"), ["ring%d" % (i + k) for k in range(4)]

    def tmp2(self):
        if self.ring_i == self.NRING - 1:
            self.ring_i = 0
        i = self.ring_i
        self.ring_i = (i + 2) % self.NRING
        return self.ringbuf[:, i:i + 2, :].rearrange("p a b -> p (a b)"), ["ring%d" % i, "ring%d" % (i + 1)]

    def define_pieces(self):
        P = []
        self.piece_gain = {}
        w_in = self.w_in
        for i in range(2):
            P.append(("mkv%d" % i, self.w_mem_kv[:, 512 * i:512 * (i + 1)], 8, 512))
            self.piece_gain["mkv%d" % i] = 2
        for nm, c0 in (("hq", C_HQ), ("hf", C_HF), ("hi", C_HI), ("hg", C_HG), ("mq", C_MQ), ("mk", C_MK),
                       ("mv", C_MV), ("mo", C_MO), ("xq", C_XQ)):
            P.append((nm, w_in[:, c0:c0 + 512], 8, 512))
            self.piece_gain[nm] = 0
        for br in range(3):
            P.append(("br%d" % br, self.w_branch[br], 4, 1024))
            for half in range(2):
                i = 2 * br + half
                P.append(("gl%d" % i, w_in[:, C_GL + 512 * i:C_GL + 512 * (i + 1)], 8, 512))
                self.piece_gain["gl%d" % i] = 0
        for i in range(2):
            P.append(("out%d" % i, self.w_out[:, 512 * i:512 * (i + 1)], 8, 512))
        for i in range(11):
            P.append(("up%d" % i, self.w_up[:, 512 * i:512 * (i + 1)], 8, 512))
            self.piece_gain["up%d" % i] = 1
        for g in range(4):
            for kh in range(2):
                P.append(("dn%d_%d" % (g, kh), self.w_down[kh * 1408:(kh + 1) * 1408, 256 * g:256 * (g + 1)], 11, 256))
        self.pieces = {p[0]: (idx,) + p[1:] for idx, p in enumerate(P)}
        self.wsc = self.dram("wscratch", [len(P), 128, 4096], BF16, kind="Internal")

    def slot_view(self, s, kc, n):
        return self.wslots[s][:, 0:kc * n].rearrange("p (k n) -> p k n", k=kc)

    def convert_weights(self):
        self.conv_names = list(self.pieces.keys())
        self.conv_i = 0

    def convert_next(self):
        ci = self.conv_i
        nm = self.conv_names[ci]
        self.conv_i += 1
        idx, src, kc, n = self.pieces[nm]
        stg, keys = self.tmp4()
        flat = stg.bitcast(BF16)[:, 0:kc * n]
        view = flat.rearrange("p (k n) -> p k n", k=kc)
        self.dma(view, src.rearrange("(k p) n -> p k n", p=128), "wcv%d" % (ci % 2), [], keys, eng="pool")
        if nm in self.piece_gain:
            gi_ = self.piece_gain[nm]
            for k_ in range(kc):
                if k_ % 2 == 0:
                    self.ts("dve", view[:, k_, :], view[:, k_, :], self.g_fm[:, gi_, k_:k_ + 1], None, ALU.mult, None, keys + ["g_fm"], keys)
                else:
                    self.act(view[:, k_, :], view[:, k_, :], AF.Copy, keys + ["g_fm"], keys, scale=self.g_fm[:, gi_, k_:k_ + 1])
        self.dma(self.wsc[idx, :, 0:kc * n], flat, "wst%d" % (ci % 2), keys, ["wsc%d" % idx])

    def load_piece(self, nm):
        idx, src, kc, n = self.pieces[nm]
        while self.conv_i < len(self.conv_names) and self.conv_i <= idx + 3:
            self.convert_next()
        s = self.slot_i
        self.slot_i = (s + 1) % len(self.wslots)
        key = "wslot%d" % s
        self.dma(self.wslots[s][:, 0:kc * n], self.wsc[idx, :, 0:kc * n], "wsl%d" % s, ["wsc%d" % idx], [key])
        return self.slot_view(s, kc, n), key

    def build(self):
        nc = self.nc
        TT = self.TT
        npr = self.n_prompt
        Tp = self.T_prompt
        Ts = self.T_sample
        self.x_prompt = self.dram("x_prompt", [npr, Tp, D])
        self.x_sample = self.dram("x_sample", [1, Ts, D])
        self.state_hgrn = self.dram("state_hgrn", [1, 4, 128, 128])
        self.state_C = self.dram("state_mlstm_C", [1, 4, 128, 128])
        self.state_n = self.dram("state_mlstm_n", [1, 4, 128])
        self.state_m = self.dram("state_mlstm_m", [1, 4])
        self.state_conv = self.dram("state_ffn_conv", [1, 2, DFF])
        self.cache_k = self.dram("cache_mem_k", [1, NMEM, 512])
        self.cache_v = self.dram("cache_mem_v", [1, NMEM, 512])
        self.mem_prompt = self.dram("mem_prompt", [npr, NMEM, D])
        self.norm1 = self.dram("norm1", [1, D])
        self.w_in = self.dram("w_in", [D, NIN])
        self.b_in = self.dram("b_in", [1, NIN])
        self.fgb = self.dram("ml_fgate_bias", [1, 4])
        self.lbl = self.dram("hg_lb_logits", [2, 512])
        self.hg_norm = self.dram("hg_norm", [1, 512])
        self.ml_norm = self.dram("ml_norm", [1, 512])
        self.mem_norm = self.dram("mem_norm", [1, D])
        self.w_mem_kv = self.dram("w_mem_kv", [D, D])
        self.w_branch = self.dram("w_branch", [3, 512, D])
        self.w_out = self.dram("w_out", [D, D])
        self.norm2 = self.dram("norm2", [1, D])
        self.w_up = self.dram("w_up", [D, 2 * DFF])
        self.conv_w = self.dram("ffn_conv_w", [3, DFF])
        self.conv_b = self.dram("ffn_conv_b", [1, DFF])
        self.w_down = self.dram("w_down", [DFF, D])
        self.final_norm = self.dram("final_norm", [1, D])
        o = "ExternalOutput"
        self.y_prompt = self.dram("y_prompt", [npr, Tp, D], kind=o)
        self.y_sample = self.dram("y_sample", [1, Ts, D], kind=o)
        self.o_hgrn_p = self.dram("hgrn_p", [npr, 4, 128, 128], kind=o)
        self.o_C_p = self.dram("mlstm_C_p", [npr, 4, 128, 128], kind=o)
        self.o_n_p = self.dram("mlstm_n_p", [npr, 4, 128], kind=o)
        self.o_m_p = self.dram("mlstm_m_p", [npr, 4], kind=o)
        self.o_mk_p = self.dram("mem_k_p", [npr, NMEM, 512], kind=o)
        self.o_mv_p = self.dram("mem_v_p", [npr, NMEM, 512], kind=o)
        self.o_cv_p = self.dram("ffn_conv_p", [npr, 2, DFF], kind=o)
        self.o_hgrn_s = self.dram("hgrn_s", [1, 4, 128, 128], kind=o)
        self.o_C_s = self.dram("mlstm_C_s", [1, 4, 128, 128], kind=o)
        self.o_n_s = self.dram("mlstm_n_s", [1, 4, 128], kind=o)
        self.o_m_s = self.dram("mlstm_m_s", [1, 4], kind=o)
        self.o_cv_s = self.dram("ffn_conv_s", [1, 2, DFF], kind=o)

        self.ps = [self.es.enter_context(nc.psum_tensor("ps%d" % i, [128, 512], F32)) for i in range(8)]
        RW = TT + 2
        self.NRING = 8
        self.RW = max(RW, 514)
        self.ringbuf = self.sb("ringbuf", [128, self.NRING, self.RW])
        self.wslots = [self.sb("wslot%d" % i, [128, 4096], BF16) for i in range(4)]
        NS = TT // 128
        self.xt = self.sb("xt", [128, NS, D])
        self.u_tm = self.sb("u_tm", [128, D], BF16)
        self.u_fm = self.sb("u_fm", [128, 8, TT], BF16)
        self.gb = self.sb("gb", [128, D])
        self.w_if = self.sb("w_if", [128, 8, 8], BF16)
        self.mhalf = self.sb("mhalf", [128, 16])
        self.one_col = self.sb("one_col", [128, 1])
        self.stg = self.sb("stg", [128, 128])
        self.carryT = self.sb("carryT", [128, 2 * NFC])
        self.Ssnap = [self.sb("Ssnap%d" % c, [128, 4, 128], BF16) for c in range(2)]
        self._rows_started = False
        self._prevTT = 0
        self.ident_f = self.sb("ident_f", [128, 128])
        self.ident_b = self.sb("ident_b", [128, 128], BF16)
        self.maskH = self.sb("maskH", [128, 128])
        self.maskM = self.sb("maskM", [128, 128])
        self.sel = self.sb("sel", [4, 4, 128])
        self.rmask = self.sb("rmask", [128, 512], BF16)
        self.ones_bf = self.sb("ones_bf", [128, 128], BF16)
        self.ones4 = self.sb("ones4", [4, 512], BF16)
        self.bfm_a = self.sb("bfm_a", [128, 32])
        self.bfm_x = self.sb("bfm_x", [128, 4])
        self.bfm_g = self.sb("bfm_g", [128, 24])
        self.bq_half = self.sb("bq_half", [128, 4])
        self.bf_neg = self.sb("bf_neg", [128, 4])
        self.bk_s = self.sb("bk_s", [128, 4])
        self.bg_half = self.sb("bg_half", [128, 24])
        self.brow_b = self.sb("brow_b", [1, 4, 512], BF16)
        self.bi = self.sb("bi", [4, 1])
        self.bfn = self.sb("bfn", [4, 2])
        self.lt = self.sb("lt", [128, 2, 4])
        self.lb = self.sb("lb", [128, 4])
        self.hgn = self.sb("hgn", [128, 512], BF16)
        self.mln = self.sb("mln", [128, 512], BF16)
        self.cw = self.sb("cw", [128, 3, NFC])
        self.cb = self.sb("cb", [128, NFC])
        self.qt = self.sb("qt", [128, 4, TT], BF16)
        self.kt = self.sb("kt", [128, 4, TT], BF16)
        self.kp = self.sb("kp", [128, 4, 128], BF16)
        self.EA = self.sb("EA", [128, 4, TT], BF16)
        self.dec = self.sb("dec", [128, 4, 16])
        self.v_tm = self.sb("v_tm", [128, NS, 512], BF16)
        self.gate_h = self.sb("gate_h", [128, NS, 512], BF16)
        self.kpT = self.sb("kpT", [128, 512], BF16)
        self.STh = self.sb("STh", [128, 128], BF16)
        self.S_f = self.sb("S_f", [128, 4, 128])
        self.S_b = self.sb("S_b", [128, 4, 128], BF16)
        self.oh_tm = self.sb("oh_tm", [128, 512], BF16)
        self.o_h_fm = self.sb("o_h_fm", [128, 4, TT], BF16)
        self.ssq = self.sb("ssq", [128, 16])
        self.g_fm = self.sb("g_fm", [128, 3, 8])
        self.ssq_h = self.sb("ssq_h", [128, 16])
        self.ssq_m = self.sb("ssq_m", [128, 8])
        self.ssq_a = self.sb("ssq_a", [128, 8])
        self.dsa = self.sb("dsa", [128, 16])
        self.mq = self.sb("mq", [128, 4, TT], BF16)
        self.mk = self.sb("mk", [128, 4, TT], BF16)
        self.vm_tm = self.sb("vm_tm", [128, NS, 4, 132], BF16)
        self.gate_m = self.sb("gate_m", [128, NS, 512], BF16)
        self.Bx = self.sb("Bx", [4, TT + 1])
        self.Gx = self.sb("Gx", [4, TT + 1])
        self.rows = self.sb("rows", [4, 4, TT])
        self.m_last = self.sb("m_last", [4, 1])
        self.sc_tm = self.sb("sc_tm", [128, 16])
        self.Wm = self.sb("Wm", [128, 4, 128])
        self.STm = self.sb("STm", [128, 4, 128], BF16)
        self.kw_tm = self.sb("kw_tm", [128, 4, 128], BF16)
        self.numI = self.sb("numI", [128, 512])
        self.num = self.sb("num", [128, 512])
        self.dsm = self.sb("dsm", [128, 32])
        self.decm = self.sb("decm", [128, 4])
        self.C_f = self.sb("C_f", [128, 4, 128])
        self.C_b = self.sb("C_b", [128, 4, 128], BF16)
        self.n_f = self.sb("n_f", [128, 4])
        self.n_b = self.sb("n_b", [128, 4], BF16)
        self.hm_tm = self.sb("hm_tm", [128, 512], BF16)
        self.h_m_fm = self.sb("h_m_fm", [128, 4, TT], BF16)
        self.xq = self.sb("xq", [128, 4, TT], BF16)
        self.KT = self.sb("KT", [128, 4, NMEM], BF16)
        self.Vb = self.sb("Vb", [128, 2, 512], BF16)
        self.pex = self.sb("pex", [128, 4, NMEM], BF16)
        self.pT = self.sb("pT", [128, 8, 128], BF16)
        self.o_x_fm = self.sb("o_x_fm", [128, 4, TT], BF16)
        self.macc = self.sb("macc", [128, 8, max(TT, 512)], BF16)
        self.merged = self.sb("merged", [128, 8, TT], BF16)
        self.kvf = self.gb[:].rearrange("p (a b) -> p a b", a=2)
        self.memn_fm = self.merged[:, :, 0:NMEM]
        self.carry = self.sb("carry", [128, NFC, 2])
        self.Gxb = [self.sb("Gxb%d" % i, [128, TT + 2]) for i in range(2)]
        self.gxi = 0

        try:
            self._program()
        except StopIteration as ex:
            print("STOPPED AT", ex)
        self.S.emit(self.es)
        return nc

    def _program(self):
        TT = self.TT
        npr, Tp, Ts = self.n_prompt, self.T_prompt, self.T_sample
        self.define_pieces()
        self.act_ch = []
        for tl, key in ((self.o_h_fm, "o_h_fm"), (self.h_m_fm, "h_m_fm"), (self.o_x_fm, "o_x_fm")):
            for i in range(4):
                self.act_ch.append((tl[:, i, :], key))
        for i in range(8):
            self.act_ch.append((self.merged[:, i, :], "merged"))
        for i in range(2):
            self.act_ch.append((self.macc[:, i, 0:TT], "macc%d" % i))
        self.setup_consts()
        self.ck("consts")
        self.convert_weights()
        self.ck("conv")

        seqs = []
        for b in range(npr):
            seqs.append(dict(kind="p", idx=b, T=Tp, TT=TT, P=128, L=64,
                             x=self.x_prompt[b], y=self.y_prompt[b]))
        seqs.append(dict(kind="s", idx=0, T=Ts, TT=Ts, P=Ts, L=Ts,
                         x=self.x_sample[0], y=self.y_sample[0]))
        for sq in seqs:
            self.seq_setup(sq)
            self.load_final_gain()
            self.ck("setup")
            for ti in range(sq["T"] // sq["TT"]):
                self.tile(sq, ti)
                self.ck("tile")
            self.seq_finish(sq)
            self.ck("seq")

    def setup_consts(self):
        K = ["consts"]
        S = self.S
        self.memset("pool", self.mhalf[:], -0.5, [], K)
        self.memset("pool", self.one_col[:], 1.0, [], K)
        self.memset("pool", self.ident_f[:], 0.0, [], K)
        S.op("pool", lambda e: e.affine_select(out=self.ident_f[:], in_=self.ident_f[:], pattern=[[-1, 128]],
                                               compare_op=ALU.not_equal, fill=1.0, base=0, channel_multiplier=1), K, K)
        self.cp("dve", self.ident_b[:], self.ident_f[:], K, K)
        self.memset("pool", self.maskH[:], 1.0, K, K)
        S.op("pool", lambda e: e.affine_select(out=self.maskH[:], in_=self.maskH[:], pattern=[[1, 128]],
                                               compare_op=ALU.is_ge, fill=0.0, base=0, channel_multiplier=-1), K, K)
        self.memset("pool", self.maskH[0:64, 64:128], 0.0, K, K)
        self.memset("pool", self.maskM[:], 0.0, K, K)
        S.op("pool", lambda e: e.affine_select(out=self.maskM[:], in_=self.maskM[:], pattern=[[1, 128]],
                                               compare_op=ALU.is_ge, fill=-1e30, base=0, channel_multiplier=-1), K, K)
        self.memset("pool", self.sel[:], 0.0, K, K)
        S.op("pool", lambda e: e.affine_select(out=self.sel[:], in_=self.sel[:], pattern=[[-1, 4], [0, 128]],
                                               compare_op=ALU.not_equal, fill=1.0, base=0, channel_multiplier=1), K, K)
        self.memset("pool", self.rmask[:], 1.0, K, K)
        self.memset("pool", self.rmask[:].rearrange("p (c l) -> p c l", l=64)[:, :, 0:1], 0.0, K, K)
        self.memset("pool", self.ones_bf[:], 1.0, K, K)
        self.memset("pool", self.ones4[:], 1.0, K, K)
        self.memset("pool", self.vm_tm[:], 1.0, K, ["vm_tm"])
        for gi_, gsrc in enumerate((self.norm1, self.norm2, self.mem_norm)):
            self.load_T(self.g_fm[:, gi_, :], gsrc[0].rearrange("(j p) -> j p", p=128), 8, [], ["g_fm"], "c0")
        b = self.b_in[0]
        self.load_T(self.bfm_a[:], b[0:4096].rearrange("(j p) -> j p", p=128), 32, [], ["bfm"], "c0")
        self.load_T(self.bfm_x[:], b[C_XQ:C_XQ + 512].rearrange("(j p) -> j p", p=128), 4, [], ["bfm"], "c0")
        self.load_T(self.bfm_g[:], b[C_GL:C_GL + 3072].rearrange("(j p) -> j p", p=128), 24, [], ["bfm"], "c0")
        for gi_, c0_ in enumerate((C_HI, C_HG, C_MV, C_MO)):
            rt, rk = self.tmp()
            self.dma(rt[0:1, 0:512], self.b_in[0:1, c0_:c0_ + 512], "c3", [], [rk])
            self.cp("dve", self.brow_b[0:1, gi_, :], rt[0:1, 0:512], [rk], K)
        self.dma(self.bi[:], self.b_in[0, C_MI:C_MI + 4].rearrange("(p o) -> p o", o=1), "c4", [], ["bi"], slow=True)
        self.dma(self.bfn[:, 0:1], self.b_in[0, C_MF:C_MF + 4].rearrange("(p o) -> p o", o=1), "c5", [], ["bfn"], slow=True)
        self.dma(self.bfn[:, 1:2], self.fgb[0].rearrange("(p o) -> p o", o=1), "c6", [], ["bfn"], slow=True)
        self.load_T(self.lt[:].rearrange("p r h -> p (r h)"), self.lbl.rearrange("r (h p) -> (r h) p", p=128), 8, [], ["lt"], "c0")
        rt, rk = self.tmp()
        self.dma(rt[:, 0:512], self.hg_norm[0].partition_broadcast(128), "c8", [], [rk])
        self.ts("dve", self.hgn[:], rt[:, 0:512], 0.5, None, ALU.mult, None, [rk], ["hgn"])
        rt, rk = self.tmp()
        self.dma(rt[:, 0:512], self.ml_norm[0].partition_broadcast(128), "c9", [], [rk])
        self.ts("dve", self.mln[:], rt[:, 0:512], 0.5, None, ALU.mult, None, [rk], ["mln"])
        self.load_T(self.cw[:].rearrange("p r f -> p (r f)"), self.conv_w.rearrange("r (f p) -> (r f) p", p=128), 3 * NFC, [], ["cw"], "c0")
        self.load_T(self.cb[:], self.conv_b[0].rearrange("(f p) -> f p", p=128), NFC, [], ["cw"], "c0")
        self.dma(self.w_if[:], self.w_in[:, C_MI:C_MI + 8].rearrange("(k p) n -> p k n", p=128), "c12", [], ["w_if"], eng="pool")
        for k_ in range(8):
            self.ts("dve", self.w_if[:, k_, :], self.w_if[:, k_, :], self.g_fm[:, 0, k_:k_ + 1], None, ALU.mult, None, ["w_if", "g_fm"], ["w_if"])
        self.ts("dve", self.bq_half[:], self.bfm_a[:, 0:4], 0.5, None, ALU.mult, None, ["bfm"], K)
        self.ts("dve", self.bf_neg[:], self.bfm_a[:, 4:8], -1.0, None, ALU.mult, None, ["bfm"], K)
        self.ts("dve", self.bk_s[:], self.bfm_a[:, 20:24], 128 ** -0.5, None, ALU.mult, None, ["bfm"], K)
        self.ts("dve", self.bg_half[:], self.bfm_g[:], 0.5, None, ALU.mult, None, ["bfm"], K)
        self.stt(self.bfn[:, 0:1], self.bfn[:, 0:1], -1.0, self.bfn[:, 1:2], ALU.mult, ALU.subtract, ["bfn"], ["bfn"])
        self.tt("dve", self.lb[:], self.lt[:, 1, :], self.lt[:, 0, :], ALU.subtract, ["lt"], ["lb"])
        self.act(self.lb[:], self.lb[:], AF.Exp, ["lb"], ["lb"])
        self.ts("dve", self.lb[:], self.lb[:], 1.0, None, ALU.add, None, ["lb"], ["lb"])
        self.S.op("dve", lambda e: e.reciprocal(out=self.lb[:], in_=self.lb[:]), ["lb"], ["lb"])

    def rstd_rows(self, src, Pn, src_keys, site=0):
        c = 4 * site
        sk = "ssq%d" % site
        jt, jk = self.tmp()
        self.act(jt[0:Pn, 0:512].bitcast(BF16), src, AF.Square, src_keys, [sk, jk], accum=self.ssq[0:Pn, c:c + 1])
        self.ts("dve", self.ssq[0:Pn, c + 1:c + 2], self.ssq[0:Pn, c:c + 1], 1.0 / D, EPS, ALU.mult, ALU.add, [sk], [sk])
        self.pow_m05(self.ssq[0:Pn, c + 2:c + 3], self.ssq[0:Pn, c + 1:c + 2], [sk], [sk])
        return self.ssq[0:Pn, c + 2:c + 3], sk

    def norm_transpose(self, src_fn, Pn, nsub, site, dst, dst_key, src_keys):
        for j in range(nsub):
            if src_keys == "provider":
                src, skeys = src_fn(j)
            else:
                src = src_fn(j)
                skeys = ["xt%d" % j] if src_keys is None else src_keys
            rs, sk = self.rstd_rows(src, Pn, skeys, site)
            self.ts("dve", self.u_tm[0:Pn, :], src, rs, None, ALU.mult, None, list(skeys) + [sk], ["u_tm"])
            pt, pk = self.psum()
            ptb = pt[:].bitcast(BF16)
            for kc in range(8):
                self.tr(ptb[:, kc * Pn:(kc + 1) * Pn], self.u_tm[0:Pn, kc * 128:(kc + 1) * 128], self.ident_b[0:Pn, 0:Pn],
                        ["u_tm", "consts"], [pk])
            self.cp("dve", dst[:, :, j * Pn:(j + 1) * Pn], ptb[:, 0:8 * Pn].rearrange("p (k t) -> p k t", k=8), [pk], [dst_key])
            self.pfree(pk)

    def seq_setup(self, sq):
        self._rows_started = False
        SK = ["S_f%d" % h for h in range(4)]
        SBK = ["S_b%d" % h for h in range(4)]
        if sq["kind"] == "p":
            b = sq["idx"]
            self.memset("pool", self.S_f[:], 0.0, [], SK)
            self.memset("pool", self.S_b[:], 0.0, [], SBK)
            self.memset("pool", self.C_f[:], 0.0, [], ["C_f"])
            self.memset("pool", self.C_b[:], 0.0, [], ["C_b"])
            self.memset("pool", self.n_f[:], 0.0, [], ["n_f"])
            self.memset("pool", self.n_b[:], 0.0, [], ["n_b"])
            self.memset("pool", self.carry[:], 0.0, [], ["carry"])
            self.memset("pool", self.Bx[:, 0:1], 0.0, [], ["Bx"])
            self.memset("pool", self.Gx[:, 0:1], 0.0, [], ["Gx"])
            memt = self.xt
            self.dma(memt[:, 0:2, :], self.mem_prompt[b].rearrange("(j p) d -> p j d", p=128), "xt0", [], ["xt0", "xt1"])
            self.ck("s_pre")
            self.norm_transpose(lambda j: memt[:, j, :], 128, 2, 3, self.memn_fm, "merged", None)
            self.ck("s_nt")
            wk, wkk = self.load_piece("mkv0")
            for h in range(4):
                pt, pk = self.psum()
                for kc in range(8):
                    self.mm(pt[:, 0:NMEM], wk[:, kc, h * 128:(h + 1) * 128], self.memn_fm[:, kc, :], kc == 0, kc == 7,
                            [wkk, "merged"], [pk])
                self.cp("act", self.KT[:, h, :], pt[:, 0:NMEM], [pk], ["KT"])
                self.pfree(pk)
            for mc in range(2):
                pt, pk = self.psum()
                for kc in range(8):
                    self.mm(pt[:, :], self.memn_fm[:, kc, mc * 128:(mc + 1) * 128], wk[:, kc, :], kc == 0, kc == 7,
                            [wkk, "merged"], [pk])
                self.cp("act", self.kvf[:, mc, :], pt[:, :], [pk], ["gb"])
                self.pfree(pk)
            self.ck("s_kt")
            self.dma(self.o_mk_p[b].rearrange("(j p) d -> p j d", p=128), self.kvf[:], "kvo", ["gb"], [], final=True)
            self.ck("s_ko")
            wv, wvk = self.load_piece("mkv1")
            for mc in range(2):
                pt, pk = self.psum()
                for kc in range(8):
                    self.mm(pt[:, :], self.memn_fm[:, kc, mc * 128:(mc + 1) * 128], wv[:, kc, :], kc == 0, kc == 7,
                            [wvk, "merged"], [pk])
                self.cp("act", self.kvf[:, mc, :], pt[:, :], [pk], ["gb"])
                self.cp("dve", self.Vb[:, mc, :], pt[:, :], [pk], ["Vb"])
                self.pfree(pk)
            self.dma(self.o_mv_p[b].rearrange("(j p) d -> p j d", p=128), self.kvf[:], "kvo", ["gb"], [], final=True)
        else:
            self.dma(self.S_f[:], self.state_hgrn[0].rearrange("h c v -> c h v"), "st0", [], SK)
            self.cp("dve", self.S_b[:], self.S_f[:], SK, SBK)
            self.dma(self.C_f[:], self.state_C[0].rearrange("h c v -> c h v"), "st1", [], ["C_f"])
            self.cp("dve", self.C_b[:], self.C_f[:], ["C_f"], ["C_b"])
            self.load_T(self.n_f[:], self.state_n[0], 4, [], ["n_f"], "st2")
            self.cp("dve", self.n_b[:], self.n_f[:], ["n_f"], ["n_b"])
            self.load_T(self.carry[:].rearrange("p f r -> p r f"), self.state_conv[0].rearrange("r (f p) -> (r f) p", p=128),
                        2 * NFC, [], ["carry"], "st2")
            self.memset("pool", self.Bx[:, 0:1], 0.0, [], ["Bx"])
            self.dma(self.Gx[:, 0:1], self.state_m[0].rearrange("(p o) -> p o", o=1), "st4", [], ["Gx"], slow=True)
            self.dma(self.kvf[:], self.cache_k[0].rearrange("(j p) d -> p j d", p=128), "st5", [], ["gb"])
            for h in range(4):
                pt, pk = self.psum()
                for mc in range(2):
                    self.tr(pt[:, mc * 128:(mc + 1) * 128], self.kvf[:, mc, h * 128:(h + 1) * 128], self.ident_f[:],
                            ["gb", "consts"], [pk])
                self.cp("act", self.KT[:, h, :], pt[:, 0:NMEM], [pk], ["KT"])
                self.pfree(pk)
            self.dma(self.kvf[:], self.cache_v[0].rearrange("(j p) d -> p j d", p=128), "st5", [], ["gb"])
            self.cp("dve", self.Vb[:], self.kvf[:], ["gb"], ["Vb"])

    def load_final_gain(self):
        self.dma(self.gb[:], self.final_norm[0].partition_broadcast(128), "gb", [], ["gb"])

    def seq_finish(self, sq):
        if sq["kind"] == "p":
            b = sq["idx"]
            oh, oc, on, om, ocv = self.o_hgrn_p[b], self.o_C_p[b], self.o_n_p[b], self.o_m_p[b], self.o_cv_p[b]
        else:
            oh, oc, on, om, ocv = self.o_hgrn_s[0], self.o_C_s[0], self.o_n_s[0], self.o_m_s[0], self.o_cv_s[0]
        TT = sq["TT"]
        SK = ["S_f%d" % h for h in range(4)]
        self.dma(oh.rearrange("h c v -> c h v"), self.S_f[:], "fo0", SK, [], final=True)
        self.dma(oc.rearrange("h c v -> c h v"), self.C_f[:], "fo1", ["C_f"], [], final=True)
        self.store_T(on, self.n_f[:], 4, ["n_f"], "fo2")
        self.dma(om.rearrange("(p o) -> p o", o=1), self.m_last[:, 0:1], "fo3", ["m_last"], [], final=True, slow=True)
        self.cp("dve", self.carryT[:].rearrange("p (r f) -> p r f", r=2), self.carry[:].rearrange("p f r -> p r f"), ["carry"], ["carryT"])
        self.store_T(ocv.rearrange("r (f p) -> (r f) p", p=128), self.carryT[:], 2 * NFC, ["carryT"], "fo2")

    def fm_group(self, w, wkey, ncol_chunks, rhs, rhs_key, TT, consume):
        KC = w.shape[1]
        for c in range(ncol_chunks):
            pt, pk = self.psum()
            for kc in range(KC):
                self.mm(pt[:, 0:TT], w[:, kc, c * 128:(c + 1) * 128], rhs[:, kc, 0:TT], kc == 0, kc == KC - 1,
                        [wkey, rhs_key], [pk])
            consume(c, pt, pk)
            self.pfree(pk)

    def tm_group(self, w, wkey, c0, P, NS, consume):
        for j in range(NS):
            pt, pk = self.psum()
            for kc in range(8):
                self.mm(pt[0:P, :], self.u_fm[:, kc, j * P:(j + 1) * P], w[:, kc, :], kc == 0, False, [wkey, "u_fm"], [pk])
            self.mm(pt[0:P, :], self.ones_bf[0:1, 0:P], self.brow_b[0:1, c0, :], False, True, ["consts"], [pk])
            consume(j, pt, pk)
            self.pfree(pk)

    def tile(self, sq, ti):
        TT, P, L = sq["TT"], sq["P"], sq["L"]
        NS = TT // P
        t0 = ti * TT
        x_ap = sq["x"][t0:t0 + TT, :].rearrange("(j p) d -> p j d", p=P)
        y_ap = sq["y"][t0:t0 + TT, :].rearrange("(j p) d -> p j d", p=P)
        xt = self.xt
        u_fm = self.u_fm
        def stage_x(j):
            xs, xk = self.tmp2()
            self.dma(xs[0:P, 0:D], x_ap[:, j, :], "xs%d" % (j % 2), [], xk)
            return xs[0:P, 0:D], xk
        self.norm_transpose(stage_x, P, NS, 0, self.u_fm, "u_fm", "provider")
        self.ck("normT")
        w, wk = self.load_piece("hq")
        self.fm_group(w, wk, 4, u_fm, "u_fm", TT, self._c_hq_factory(TT))
        w, wk = self.load_piece("hf")
        self.fm_group(w, wk, 4, u_fm, "u_fm", TT, self._c_hf_factory(TT, L))
        w, wk = self.load_piece("hi")
        self.tm_group(w, wk, 0, P, NS,
                      lambda j, pt, pk: self.cp("act", self.v_tm[0:P, j, :], pt[0:P, :], [pk], ["v_tm"]))
        w, wk = self.load_piece("hg")

        def c_hg(j, pt, pk):
            xb, xk = self.tmp()
            th, tk = self.tmp()
            self.cp("act", xb[0:P, 0:512], pt[0:P, :], [pk], [xk])
            self.act(th[0:P, 0:512], pt[0:P, :], AF.Tanh, [pk], [tk], scale=0.5)
            self.stt(xb[0:P, 0:512], th[0:P, 0:512], 1.0, xb[0:P, 0:512], ALU.add, ALU.mult, [xk, tk], [xk])
            self.tt("dve", self.gate_h[0:P, j, :], xb[0:P, 0:512], self.hgn[0:P, :], ALU.mult, [xk, "hgn"], ["gate_h"])
        self.tm_group(w, wk, 1, P, NS, c_hg)
        self.ck("hgates")
        for j in range(NS):
            self.hgrn_subtile(sq, j)
        self.ck("hgrn")
        w, wk = self.load_piece("mq")
        self.fm_group(w, wk, 4, u_fm, "u_fm", TT,
                      lambda h, pt, pk: self.act(self.mq[:, h, 0:TT], pt[:, 0:TT], AF.Identity, [pk, "bfm"], ["mq"],
                                                 bias=self.bfm_a[:, 16 + h:17 + h]))
        w, wk = self.load_piece("mk")
        self.fm_group(w, wk, 4, u_fm, "u_fm", TT,
                      lambda h, pt, pk: self.act(self.mk[:, h, 0:TT], pt[:, 0:TT], AF.Identity, [pk, "consts"], ["mk"],
                                                 bias=self.bk_s[:, h:h + 1], scale=128 ** -0.5))
        w, wk = self.load_piece("mv")
        self.tm_group(w, wk, 2, P, NS,
                      lambda j, pt, pk: self.cp("act", self.vm_tm[0:P, j, :, 0:128],
                                                pt[0:P, :].rearrange("p (h v) -> p h v", h=4), [pk], ["vm_tm"]))
        w, wk = self.load_piece("mo")

        def c_mo(j, pt, pk):
            th, tk = self.tmp()
            self.act(th[0:P, 0:512], pt[0:P, :], AF.Tanh, [pk], [tk], scale=0.5)
            self.stt(self.gate_m[0:P, j, :], th[0:P, 0:512], 1.0, self.mln[0:P, :], ALU.add, ALU.mult, [tk, "mln"], ["gate_m"])
        self.tm_group(w, wk, 3, P, NS, c_mo)
        self.ck("mproj")
        self.mlstm_rows(sq)
        self.ck("mrows")
        for j in range(NS):
            self.mlstm_subtile(sq, j)
        self.ck("mlstm")
        w, wk = self.load_piece("xq")
        self.fm_group(w, wk, 4, u_fm, "u_fm", TT,
                      lambda h, pt, pk: self.act(self.xq[:, h, 0:TT], pt[:, 0:TT], AF.Identity, [pk, "bfm"], ["xq"],
                                                 bias=self.bfm_x[:, h:h + 1]))
        for j in range(NS):
            self.attn_subtile(sq, j)
        self.ck("attn")
        srcs = [(self.o_h_fm, "o_h_fm"), (self.h_m_fm, "h_m_fm"), (self.o_x_fm, "o_x_fm")]
        for br in range(3):
            wb, wbk = self.load_piece("br%d" % br)
            src, srck = srcs[br]
            for half in range(2):
                wg, wgk = self.load_piece("gl%d" % (2 * br + half))
                for c in range(4):
                    oc = half * 4 + c
                    pg, pgk = self.psum()
                    for kc in range(8):
                        self.mm(pg[:, 0:TT], wg[:, kc, c * 128:(c + 1) * 128], u_fm[:, kc, 0:TT], kc == 0, kc == 7,
                                [wgk, "u_fm"], [pgk])
                    pp, ppk = self.psum()
                    for kc in range(4):
                        self.mm(pp[:, 0:TT], wb[:, kc, oc * 128:(oc + 1) * 128], src[:, kc, 0:TT], kc == 0, kc == 3,
                                [wbk, srck], [ppk])
                    th, tk = self.tmp()
                    gi = br * 8 + oc
                    self.act(th[:, 0:TT], pg[:, 0:TT], AF.Tanh, [pgk, "consts"], [tk], bias=self.bg_half[:, gi:gi + 1], scale=0.5)
                    self.pfree(pgk)
                    mk_ = "macc%d" % oc
                    if br == 0:
                        self.stt(self.macc[:, oc, 0:TT], th[:, 0:TT], 1.0, pp[:, 0:TT], ALU.add, ALU.mult, [tk, ppk], [mk_])
                    else:
                        self.stt(th[:, 0:TT], th[:, 0:TT], 1.0, pp[:, 0:TT], ALU.add, ALU.mult, [tk, ppk], [tk])
                        if br == 1:
                            self.tt("dve", self.macc[:, oc, 0:TT], self.macc[:, oc, 0:TT], th[:, 0:TT], ALU.add, [mk_, tk], [mk_])
                        else:
                            self.tt("dve", self.merged[:, oc, 0:TT], self.macc[:, oc, 0:TT], th[:, 0:TT], ALU.add, [mk_, tk], ["merged"])
                    self.pfree(ppk)
        self.ck("merge")
        for j in range(NS):
            self.dma(xt[0:P, j, :], x_ap[:, j, :], "xt%d" % j, [], ["xt%d" % j])
        for i in range(2):
            w, wk = self.load_piece("out%d" % i)
            for j in range(NS):
                pt, pk = self.psum()
                for kc in range(8):
                    self.mm(pt[0:P, :], self.merged[:, kc, j * P:(j + 1) * P], w[:, kc, :], kc == 0, kc == 7, [wk, "merged"], [pk])
                self.stt(xt[0:P, j, i * 512:(i + 1) * 512], pt[0:P, :], 0.5, xt[0:P, j, i * 512:(i + 1) * 512],
                         ALU.mult, ALU.add, [pk, "xt%d" % j], ["xt%d" % j])
                self.pfree(pk)
        self.norm_transpose(lambda j: xt[0:P, j, :], P, NS, 1, self.u_fm, "u_fm", None)
        self.ck("norm2")
        up_cache = {}

        def up_chunk(colchunk):
            pi, c = divmod(colchunk, 4)
            if pi not in up_cache:
                up_cache.clear()
                up_cache[pi] = self.load_piece("up%d" % pi)
            w, wk = up_cache[pi]
            pt, pk = self.psum()
            for kc in range(8):
                self.mm(pt[:, 0:TT], w[:, kc, c * 128:(c + 1) * 128], u_fm[:, kc, 0:TT], kc == 0, kc == 7, [wk, "u_fm"], [pk])
            return pt, pk
        for fc in range(NFC):
            pt, pk = up_chunk(fc)
            self.ffn_gate(sq, fc, pt, pk, TT)
            self.pfree(pk)
        for fc in range(NFC):
            pt, pk = up_chunk(NFC + fc)
            at, ak = self.act_ch[fc]
            self.tt("dve", at[:, 0:TT], at[:, 0:TT], pt[:, 0:TT], ALU.mult, [ak, pk], [ak])
            self.pfree(pk)
        self.ck("ffnup")
        for g in range(4):
            banks = [self.psum() for _ in range(NS)]
            for kh in range(2):
                w, wk = self.load_piece("dn%d_%d" % (g, kh))
                for j in range(NS):
                    pt, pk = banks[j]
                    for kc in range(11):
                        self.mm(pt[0:P, 0:256], self.act_ch[kh * 11 + kc][0][:, j * P:(j + 1) * P], w[:, kc, :],
                                kh == 0 and kc == 0, kh == 1 and kc == 10, [wk, self.act_ch[kh * 11 + kc][1]], [pk])
            for j in range(NS):
                pt, pk = banks[j]
                self.stt(xt[0:P, j, g * 256:(g + 1) * 256], pt[0:P, 0:256], 0.5, xt[0:P, j, g * 256:(g + 1) * 256],
                         ALU.mult, ALU.add, [pk, "xt%d" % j], ["xt%d" % j])
                self.pfree(pk)
        self.ck("ffndn")
        for j in range(NS):
            src = xt[0:P, j, :]
            xk = "xt%d" % j
            rs, sk = self.rstd_rows(src, P, [xk], 2)
            self.stt(src, src, rs, self.gb[0:P, :], ALU.mult, ALU.mult, [xk, sk, "gb"], [xk])
            self.dma(y_ap[:, j, :], src, "yo%d" % j, [xk], [], final=True)

    def _c_hq_factory(self, TT):
        def c_hq(h, pt, pk):
            xb, xk = self.tmp()
            th, tk = self.tmp()
            self.act(xb[:, 0:TT], pt[:, 0:TT], AF.Identity, [pk, "bfm"], [xk], bias=self.bfm_a[:, h:h + 1])
            self.act(th[:, 0:TT], pt[:, 0:TT], AF.Tanh, [pk, "consts"], [tk], bias=self.bq_half[:, h:h + 1], scale=0.5)
            self.stt(self.qt[:, h, 0:TT], th[:, 0:TT], 1.0, xb[:, 0:TT], ALU.add, ALU.mult, [xk, tk], ["qt"])
        return c_hq

    def _c_hf_factory(self, TT, L):
        def c_hf(h, pt, pk):
            e_, ek = self.tmp()
            l1, l1k = self.tmp()
            A_, Ak = self.tmp()
            en, enk = self.tmp()
            l2, l2k = e_, ek
            f_, fk = e_, ek
            self.act(e_[:, 0:TT], pt[:, 0:TT], AF.Exp, [pk, "consts"], [ek], bias=self.bf_neg[:, h:h + 1], scale=-1.0)
            self.act(l1[:, 0:TT], e_[:, 0:TT], AF.Ln, [ek, "lb"], [l1k], bias=self.one_col[:, 0:1], scale=self.lb[:, h:h + 1])
            self.act(l2[:, 0:TT], e_[:, 0:TT], AF.Ln, [ek], [l2k], bias=self.one_col[:, 0:1])
            self.tt("dve", l1[:, 0:TT], l1[:, 0:TT], l2[:, 0:TT], ALU.subtract, [l1k, l2k], [l1k])
            self.S.op("dve", lambda e: e.tensor_tensor_scan(out=A_[:, 0:TT], data0=self.rmask[:, 0:TT], data1=l1[:, 0:TT],
                                                            initial=0.0, op0=ALU.mult, op1=ALU.add), [l1k, "consts"], [Ak])
            self.act(f_[:, 0:TT], l1[:, 0:TT], AF.Exp, [l1k], [fk])
            self.ts("dve", f_[:, 0:TT], f_[:, 0:TT], -1.0, 1.0, ALU.mult, ALU.add, [fk], [fk])
            self.act(en[:, 0:TT], A_[:, 0:TT], AF.Exp, [Ak], [enk], scale=-1.0)
            self.act(self.EA[:, h, 0:TT], A_[:, 0:TT], AF.Exp, [Ak], ["EA"])
            nch = TT // L
            self.act(self.dec[:, h, 0:nch].rearrange("p (c o) -> p c o", o=1),
                     A_[:, 0:TT].rearrange("p (c l) -> p c l", l=L)[:, :, L - 1:L], AF.Exp, [Ak], ["dec"])
            self.tt("dve", self.kt[:, h, 0:TT], f_[:, 0:TT], en[:, 0:TT], ALU.mult, [fk, enk], ["kt"])
            self.stt(self.qt[:, h, 0:TT], self.qt[:, h, 0:TT], 0.5, self.EA[:, h, 0:TT], ALU.mult, ALU.mult, ["qt", "EA"], ["qt"])
        return c_hf

    def hgrn_subtile(self, sq, j):
        P, L = sq["P"], sq["L"]
        NCH = P // L
        c0 = j * P
        pt, pk = self.psum()
        ptb = pt[:].bitcast(BF16)
        for h in range(4):
            for c in range(NCH):
                cg = (c0 + c * L) // L
                self.ts("pool", self.kp[:, h, c * L:(c + 1) * L], self.kt[:, h, c0 + c * L:c0 + (c + 1) * L],
                        self.dec[:, h, cg:cg + 1], None, ALU.mult, None, ["kt", "dec"], ["kp"])
        for h in range(4):
            self.tr(ptb[0:P, h * 128:(h + 1) * 128], self.kp[:, h, 0:P], self.ident_b[:], ["kp", "consts"], [pk])
        self.cp("act", self.kpT[0:P, :], ptb[0:P, 0:512], [pk], ["kpT"])
        self.pfree(pk)
        po, pok = self.psum()
        for h in range(4):
            ps_, psk = self.psum()
            self.mm(ps_[0:P, 0:P], self.kt[:, h, c0:c0 + P], self.qt[:, h, c0:c0 + P], True, True, ["kt", "qt"], [psk])
            self.tt("dve", self.STh[0:P, 0:P], ps_[0:P, 0:P], self.maskH[0:P, 0:P], ALU.mult, [psk, "consts"], ["STh"])
            self.pfree(psk)
            sbk = "S_b%d" % h
            sfk = "S_f%d" % h
            snaps = []
            for c in range(NCH):
                r0 = c * L
                snap = self.Ssnap[c][:, h, :]
                snk = "Ssnap%d_%d" % (c, h)
                self.cp("pool", snap, self.S_b[:, h, :], [sbk], [snk])
                snaps.append((snap, snk))
                pu, puk = self.psum()
                self.mm(pu[:, 0:128], self.kpT[r0:r0 + L, h * 128:(h + 1) * 128], self.v_tm[r0:r0 + L, j, h * 128:(h + 1) * 128],
                        True, True, ["kpT", "v_tm"], [puk])
                cg = (c0 + c * L) // L
                self.stt(self.S_f[:, h, :], self.S_f[:, h, :], self.dec[:, h, cg:cg + 1], pu[:, 0:128], ALU.mult, ALU.add,
                         [sfk, "dec", puk], [sfk])
                self.pfree(puk)
                self.cp("act", self.S_b[:, h, :], self.S_f[:, h, :], [sfk], [sbk])
            self.mm(po[0:P, h * 128:(h + 1) * 128], self.STh[0:P, 0:P], self.v_tm[0:P, j, h * 128:(h + 1) * 128], True, False,
                    ["STh", "v_tm"], [pok])
            for c in range(NCH):
                r0 = c * L
                snap, snk = snaps[c]
                self.mm(po[r0:r0 + L, h * 128:(h + 1) * 128], self.qt[:, h, c0 + r0:c0 + r0 + L], snap, False, True,
                        ["qt", snk], [pok])
        for h in range(4):
            jt, jk = self.tmp()
            self.act(jt[0:P, 0:128], po[0:P, h * 128:(h + 1) * 128], AF.Square, [pok], ["ssq_h", jk],
                     accum=self.ssq_h[0:P, 4 + h:5 + h])
        self.ts("dve", self.ssq_h[0:P, 8:12], self.ssq_h[0:P, 4:8], 1.0 / 128, EPS, ALU.mult, ALU.add, ["ssq_h"], ["ssq_h"])
        self.pow_m05(self.ssq_h[0:P, 12:16], self.ssq_h[0:P, 8:12], ["ssq_h"], ["ssq_h"])
        for h in range(4):
            self.stt(self.oh_tm[0:P, h * 128:(h + 1) * 128], po[0:P, h * 128:(h + 1) * 128], self.ssq_h[0:P, 12 + h:13 + h],
                     self.gate_h[0:P, j, h * 128:(h + 1) * 128], ALU.mult, ALU.mult, [pok, "ssq_h", "gate_h"], ["oh_tm"])
        self.pfree(pok)
        self.tm_to_fm(self.oh_tm, "oh_tm", P, self.o_h_fm, "o_h_fm", c0)

    def tm_to_fm(self, src, srck, P, dst, dstk, c0):
        pt, pk = self.psum()
        ptb = pt[:].bitcast(BF16)
        for h in range(4):
            self.tr(ptb[:, h * P:(h + 1) * P], src[0:P, h * 128:(h + 1) * 128], self.ident_b[0:P, 0:P], [srck, "consts"], [pk])
        self.cp("act", dst[:, :, c0:c0 + P], ptb[:, 0:4 * P].rearrange("p (h t) -> p h t", h=4), [pk], [dstk])
        self.pfree(pk)

    def mlstm_rows(self, sq):
        TT = sq["TT"]
        R = self.rows
        sp, Dr, a_, mt = (R[:, i, 0:TT] for i in range(4))
        nG, et = sp, mt
        K = ["rows"]
        pi, pik = self.psum()
        for kc in range(8):
            self.mm(pi[0:4, 0:TT], self.w_if[:, kc, 0:4], self.u_fm[:, kc, 0:TT], kc == 0, kc == 7, ["w_if", "u_fm"], [pik])
        pf, pfk = self.psum()
        for kc in range(8):
            self.mm(pf[0:4, 0:TT], self.w_if[:, kc, 4:8], self.u_fm[:, kc, 0:TT], kc == 0, kc == 7, ["w_if", "u_fm"], [pfk])
        if self._rows_started:
            pT = self._prevTT
            self.cp("dve", self.Bx[:, 0:1], self.Bx[:, pT:pT + 1], ["Bx"], ["Bx"])
            self.cp("dve", self.Gx[:, 0:1], self.Gx[:, pT:pT + 1], ["Gx"], ["Gx"])
        self._rows_started = True
        self._prevTT = TT
        self.act(sp, pf[0:4, 0:TT], AF.Exp, [pfk, "bfn"], K, bias=self.bfn[:, 0:1], scale=-1.0)
        self.pfree(pfk)
        self.act(sp, sp, AF.Ln, K, K, bias=self.one_col[0:4, 0:1])
        self.S.op("dve", lambda e: e.tensor_tensor_scan(out=self.Bx[:, 1:TT + 1], data0=self.ones4[:, 0:TT], data1=sp,
                                                        initial=self.Bx[:, 0:1], op0=ALU.mult, op1=ALU.subtract),
                  K + ["Bx", "consts"], ["Bx"])
        self.stt(Dr, pi[0:4, 0:TT], self.bi[:, 0:1], self.Bx[:, 1:TT + 1], ALU.add, ALU.subtract, [pik, "bi", "Bx"] + K, K)
        self.pfree(pik)
        self.S.op("dve", lambda e: e.tensor_tensor_scan(out=self.Gx[:, 1:TT + 1], data0=self.ones4[:, 0:TT], data1=Dr,
                                                        initial=self.Gx[:, 0:1], op0=ALU.mult, op1=ALU.max),
                  K + ["Gx", "consts"], ["Gx"])
        self.ts("dve", nG, self.Gx[:, 1:TT + 1], -1.0, None, ALU.mult, None, ["Gx"] + K, K)
        self.tt("dve", mt, self.Bx[:, 1:TT + 1], self.Gx[:, 1:TT + 1], ALU.add, ["Bx", "Gx"] + K, K)
        self.cp("dve", self.m_last[:, 0:1], R[:, 3, TT - 1:TT], K, ["m_last"])
        self.act(et, mt, AF.Exp, K, K, scale=-1.0)

    def mlstm_subtile(self, sq, j):
        P = sq["P"]
        c0 = j * P
        R = self.rows
        K = ["rows"]
        self.act(R[:, 2, c0:c0 + P], R[:, 0, c0:c0 + P], AF.Exp, K + ["Gx"], K, bias=self.Gx[:, c0:c0 + 1])
        pt, pk = self.psum()
        for q, ri in enumerate((1, 2, 3)):
            self.tr(pt[0:P, 4 * q:4 * q + 4], R[:, ri, c0:c0 + P], self.ident_f[0:4, 0:4], K + ["consts"], [pk])
        self.cp("dve", self.sc_tm[0:P, 0:12], pt[0:P, 0:12], [pk], ["sc_tm"])
        self.pfree(pk)
        pd, pdk = self.psum()
        for h in range(4):
            self.mm(pd[:, h:h + 1], self.sel[:, h, :], R[:, 2, c0 + P - 1:c0 + P], True, True, K + ["consts"], [pdk])
        self.cp("dve", self.decm[:, :], pd[:, 0:4], [pdk], ["decm"])
        self.pfree(pdk)
        pg, pgk = self.psum()
        for h in range(4):
            self.mm(pg[0:P, h * 128:h * 128 + P], self.sel[:, h, 0:P], R[:, 0, c0:c0 + P], True, False, K + ["consts"], [pgk])
            self.mm(pg[0:P, h * 128:h * 128 + P], self.ident_f[0:P, 0:P], self.maskM[0:P, 0:P], False, True, ["consts"], [pgk])
        for h in range(4):
            self.act(self.Wm[0:P, h, 0:P], pg[0:P, h * 128:h * 128 + P], AF.Exp, [pgk, "sc_tm"], ["Wm"], bias=self.sc_tm[0:P, h:h + 1])
        self.pfree(pgk)
        pq, pqk = self.psum()
        for h in range(4):
            self.mm(pq[0:P, h * 128:h * 128 + P], self.mk[:, h, c0:c0 + P], self.mq[:, h, c0:c0 + P], True, True, ["mk", "mq"], [pqk])
        for h in range(4):
            self.tt("dve", self.STm[0:P, h, 0:P], pq[0:P, h * 128:h * 128 + P], self.Wm[0:P, h, 0:P], ALU.mult, [pqk, "Wm"], ["STm"])
        self.pfree(pqk)
        pk2, pk2k = self.psum()
        pk2b = pk2[:].bitcast(BF16)
        for h in range(4):
            self.tr(pk2b[0:P, h * 128:(h + 1) * 128], self.mk[:, h, c0:c0 + P], self.ident_b[:], ["mk", "consts"], [pk2k])
        for h in range(4):
            self.ts("dve", self.kw_tm[0:P, h, :], pk2b[0:P, h * 128:(h + 1) * 128], self.Wm[0:P, h, P - 1:P], None, ALU.mult, None,
                    [pk2k, "Wm"], ["kw_tm"])
        self.pfree(pk2k)
        pa, pak = self.psum()
        pb, pbk = self.psum()
        pdn, pdnk = self.psum()
        for h in range(4):
            self.mm(pa[0:P, h * 128:(h + 1) * 128], self.STm[0:P, h, 0:P], self.vm_tm[0:P, j, h, 0:128], True, True, ["STm", "vm_tm"], [pak])
        for h in range(4):
            self.mm(pb[0:P, h * 128:(h + 1) * 128], self.mq[:, h, c0:c0 + P], self.C_b[:, h, :], True, True, ["mq", "C_b"], [pbk])
        for h in range(4):
            self.mm(pdn[0:P, h:h + 1], self.STm[0:P, h, 0:P], self.vm_tm[0:P, j, h, 128:129], True, True, ["STm", "vm_tm"], [pdnk])
        for h in range(4):
            self.mm(pdn[0:P, 4 + h:5 + h], self.mq[:, h, c0:c0 + P], self.n_b[:, h:h + 1], True, True, ["mq", "n_b"], [pdnk])
        for h in range(4):
            self.act(self.numI[0:P, h * 128:(h + 1) * 128], pb[0:P, h * 128:(h + 1) * 128], AF.Identity, [pbk, "sc_tm"], ["numI"],
                     scale=self.sc_tm[0:P, 4 + h:5 + h])
        self.pfree(pbk)
        self.tt("dve", self.num[0:P, :], pa[0:P, :], self.numI[0:P, :], ALU.add, [pak, "numI"], ["num"])
        self.pfree(pak)
        ds = self.dsm
        Kd = ["dsm"]
        self.cp("dve", ds[0:P, 0:8], pdn[0:P, 0:8], [pdnk] + Kd, Kd)
        self.pfree(pdnk)
        self.tt("dve", ds[0:P, 8:12], ds[0:P, 4:8], self.sc_tm[0:P, 4:8], ALU.mult, Kd + ["sc_tm"], Kd)
        self.tt("dve", ds[0:P, 8:12], ds[0:P, 8:12], ds[0:P, 0:4], ALU.add, Kd, Kd)
        self.ts("dve", ds[0:P, 12:16], ds[0:P, 8:12], -1.0, None, ALU.mult, None, Kd, Kd)
        self.tt("dve", ds[0:P, 12:16], ds[0:P, 12:16], ds[0:P, 8:12], ALU.max, Kd, Kd)
        self.tt("dve", ds[0:P, 12:16], ds[0:P, 12:16], self.sc_tm[0:P, 8:12], ALU.max, Kd + ["sc_tm"], Kd)
        self.S.op("dve", lambda e: e.reciprocal(out=ds[0:P, 16:20], in_=ds[0:P, 12:16]), Kd, Kd)
        for h in range(4):
            jt, jk = self.tmp()
            self.act(jt[0:P, 0:128], self.num[0:P, h * 128:(h + 1) * 128], AF.Square, ["num"], ["ssq_m", jk],
                     accum=self.ssq_m[0:P, h:h + 1])
        self.tt("dve", ds[0:P, 20:24], ds[0:P, 16:20], ds[0:P, 16:20], ALU.mult, Kd, Kd)
        self.tt("dve", ds[0:P, 20:24], ds[0:P, 20:24], self.ssq_m[0:P, 0:4], ALU.mult, Kd + ["ssq_m"], Kd)
        self.ts("dve", ds[0:P, 20:24], ds[0:P, 20:24], 1.0 / 128, EPS, ALU.mult, ALU.add, Kd, Kd)
        self.pow_m05(ds[0:P, 24:28], ds[0:P, 20:24], Kd, Kd)
        self.tt("dve", ds[0:P, 28:32], ds[0:P, 24:28], ds[0:P, 16:20], ALU.mult, Kd, Kd)
        for h in range(4):
            self.stt(self.hm_tm[0:P, h * 128:(h + 1) * 128], self.num[0:P, h * 128:(h + 1) * 128], ds[0:P, 28 + h:29 + h],
                     self.gate_m[0:P, j, h * 128:(h + 1) * 128], ALU.mult, ALU.mult, ["num", "dsm", "gate_m"], ["hm_tm"])
        self.tm_to_fm(self.hm_tm, "hm_tm", P, self.h_m_fm, "h_m_fm", c0)
        pus = []
        for h in range(4):
            if h % 2 == 0:
                pu, puk = self.psum()
            off = (h % 2) * 256
            self.mm(pu[:, off:off + 129], self.kw_tm[0:P, h, :], self.vm_tm[0:P, j, h, 0:129], True, True, ["kw_tm", "vm_tm"], [puk])
            pus.append((pu, puk, off))
        for h in range(4):
            pu, puk, off = pus[h]
            self.stt(self.C_f[:, h, :], self.C_f[:, h, :], self.decm[:, h:h + 1], pu[:, off:off + 128], ALU.mult, ALU.add,
                     ["C_f", "decm", puk], ["C_f"])
            self.stt(self.n_f[:, h:h + 1], self.n_f[:, h:h + 1], self.decm[:, h:h + 1], pu[:, off + 128:off + 129], ALU.mult, ALU.add,
                     ["n_f", "decm", puk], ["n_f"])
            if h % 2 == 1:
                self.pfree(puk)
        self.cp("act", self.C_b[:], self.C_f[:], ["C_f"], ["C_b"])
        self.cp("act", self.n_b[:], self.n_f[:], ["n_f"], ["n_b"])

    def attn_subtile(self, sq, j):
        P = sq["P"]
        c0 = j * P
        sc = 128 ** -0.5
        banks = [self.psum(), self.psum()]
        for h in range(4):
            pl, plk = banks[h // 2]
            off = (h % 2) * NMEM
            self.mm(pl[0:P, off:off + NMEM], self.xq[:, h, c0:c0 + P], self.KT[:, h, :], True, True, ["xq", "KT"], [plk])
        ds = self.dsa
        for hh in range(2):
            pl, plk = banks[hh]
            self.S.op("dve", lambda e, pl=pl, hh=hh: e.tensor_reduce(out=ds[0:P, 2 * hh:2 * hh + 2],
                                                                     in_=pl[0:P, :].rearrange("p (h m) -> p h m", h=2),
                                                                     axis=AX.X, op=ALU.max), [plk, "dsa"], ["dsa"])
        self.ts("dve", ds[0:P, 4:8], ds[0:P, 0:4], -sc, None, ALU.mult, None, ["dsa"], ["dsa"])
        for h in range(4):
            pl, plk = banks[h // 2]
            off = (h % 2) * NMEM
            self.act(self.pex[0:P, h, :], pl[0:P, off:off + NMEM], AF.Exp, [plk, "dsa"], ["pex", "ssq_a"], bias=ds[0:P, 4 + h:5 + h], scale=sc,
                     accum=self.ssq_a[0:P, h:h + 1])
        self.pfree(banks[0][1])
        self.pfree(banks[1][1])
        self.S.op("dve", lambda e: e.reciprocal(out=ds[0:P, 8:12], in_=self.ssq_a[0:P, 0:4]), ["ssq_a", "dsa"], ["dsa"])
        for h in range(4):
            self.ts("dve", self.pex[0:P, h, :], self.pex[0:P, h, :], ds[0:P, 8 + h:9 + h], None, ALU.mult, None, ["pex", "dsa"], ["pex"])
        pt, pk = self.psum()
        ptb = pt[:].bitcast(BF16)
        for h in range(4):
            for mc in range(2):
                q = h * 2 + mc
                self.tr(ptb[:, q * P:(q + 1) * P], self.pex[0:P, h, mc * 128:(mc + 1) * 128], self.ident_b[0:P, 0:P], ["pex", "consts"], [pk])
        self.cp("act", self.pT[:, :, 0:P], ptb[:, 0:8 * P].rearrange("p (q t) -> p q t", q=8), [pk], ["pT"])
        self.pfree(pk)
        po, pok = self.psum()
        for h in range(4):
            for mc in range(2):
                self.mm(po[:, h * P:(h + 1) * P], self.Vb[:, mc, h * 128:(h + 1) * 128], self.pT[:, h * 2 + mc, 0:P], mc == 0, mc == 1,
                        ["Vb", "pT"], [pok])
        self.cp("dve", self.o_x_fm[:, :, c0:c0 + P], po[:, 0:4 * P].rearrange("p (h t) -> p h t", h=4), [pok], ["o_x_fm"])
        self.pfree(pok)

    def ffn_gate(self, sq, fc, pt, pk, TT):
        gx = self.Gxb[self.gxi]
        gk = "Gxb%d" % self.gxi
        self.gxi = (self.gxi + 1) % 2
        ck = "carry"
        self.cp("act", gx[:, 2:TT + 2], pt[:, 0:TT], [pk], [gk])
        self.cp("pool", gx[:, 0:2], self.carry[:, fc, :], [ck, gk], [gk])
        self.cp("pool", self.carry[:, fc, :], gx[:, TT:TT + 2], [gk, ck], [ck])
        cv, cvk = self.tmp()
        x2, x2k = self.tmp()
        self.ts("dve", cv[:, 0:TT], gx[:, 2:TT + 2], self.cw[:, 2, fc:fc + 1], self.cb[:, fc:fc + 1], ALU.mult, ALU.add, [gk, "cw"], [cvk])
        self.stt(cv[:, 0:TT], gx[:, 1:TT + 1], self.cw[:, 1, fc:fc + 1], cv[:, 0:TT], ALU.mult, ALU.add, [gk, "cw", cvk], [cvk])
        self.stt(cv[:, 0:TT], gx[:, 0:TT], self.cw[:, 0, fc:fc + 1], cv[:, 0:TT], ALU.mult, ALU.add, [gk, "cw", cvk], [cvk])
        self.act(x2[:, 0:TT], cv[:, 0:TT], AF.Square, [cvk], [x2k])
        self.ts("pool", x2[:, 0:TT], x2[:, 0:TT], 0.044715, 1.0, ALU.mult, ALU.add, [x2k], [x2k])
        self.tt("pool", x2[:, 0:TT], x2[:, 0:TT], cv[:, 0:TT], ALU.mult, [x2k, cvk], [x2k])
        self.act(x2[:, 0:TT], x2[:, 0:TT], AF.Tanh, [x2k], [x2k], scale=0.7978845608028654)
        at, ak = self.act_ch[fc]
        self.stt(at[:, 0:TT], x2[:, 0:TT], 1.0, cv[:, 0:TT], ALU.add, ALU.mult, [x2k, cvk], [ak])


_CACHE = {}


def _get_nc():
    if "nc" not in _CACHE:
        b = Builder(TT=512)
        _CACHE["b"] = b
        _CACHE["nc"] = b.build()
    return _CACHE["nc"]


def kernel(**inputs):
    n = 8
    nc = _get_nc()
    f = lambda a: np.ascontiguousarray(np.asarray(a, dtype=np.float32))
    in_maps = []
    for c in range(n):
        m = {
            "x_prompt": f(inputs["x_prompt"][2 * c:2 * c + 2]),
            "x_sample": f(inputs["x_sample"][c:c + 1]),
            "state_hgrn": f(inputs["state_hgrn"][0, c:c + 1]),
            "state_mlstm_C": f(inputs["state_mlstm_C"][0, c:c + 1]),
            "state_mlstm_n": f(inputs["state_mlstm_n"][0, c:c + 1]),
            "state_mlstm_m": f(inputs["state_mlstm_m"][0, c:c + 1]),
            "state_ffn_conv": f(inputs["state_ffn_conv"][0, c:c + 1]),
            "cache_mem_k": f(inputs["cache_mem_k"][0, c:c + 1]).reshape(1, NMEM, 512),
            "cache_mem_v": f(inputs["cache_mem_v"][0, c:c + 1]).reshape(1, NMEM, 512),
            "mem_prompt": f(inputs["mem_prompt"][2 * c:2 * c + 2]),
            "norm1": f(inputs["norm1"]),
            "w_in": f(inputs["w_in"][0]),
            "b_in": f(inputs["b_in"]),
            "ml_fgate_bias": f(inputs["ml_fgate_bias"]),
            "hg_lb_logits": f(inputs["hg_lb_logits"]),
            "hg_norm": f(inputs["hg_norm"]),
            "ml_norm": f(inputs["ml_norm"]),
            "mem_norm": f(inputs["mem_norm"]),
            "w_mem_kv": f(inputs["w_mem_kv"][0]),
            "w_branch": f(inputs["w_branch"][0]),
            "w_out": f(inputs["w_out"][0]),
            "norm2": f(inputs["norm2"]),
            "w_up": f(inputs["w_up"][0]),
            "ffn_conv_w": f(inputs["ffn_conv_w"][0]),
            "ffn_conv_b": f(inputs["ffn_conv_b"]),
            "w_down": f(inputs["w_down"][0]),
            "final_norm": f(inputs["final_norm"]).reshape(1, D),
        }
        in_maps.append(m)
    res = run_bass_kernel_spmd(nc, in_maps, core_ids=list(range(n)))
    R = res.results
    cat = lambda k: np.concatenate([np.asarray(r[k]) for r in R], axis=0)
    outs = (
        cat("y_prompt"), cat("y_sample"),
        cat("hgrn_p")[None], cat("mlstm_C_p")[None], cat("mlstm_n_p")[None], cat("mlstm_m_p")[None],
        cat("mem_k_p").reshape(1, 16, NMEM, 4, 128), cat("mem_v_p").reshape(1, 16, NMEM, 4, 128),
        cat("ffn_conv_p")[None],
        cat("hgrn_s")[None], cat("mlstm_C_s")[None], cat("mlstm_n_s")[None], cat("mlstm_m_s")[None],
        cat("ffn_conv_s")[None],
    )
    return tuple(np.ascontiguousarray(o, dtype=np.float32) for o in outs)
```

```python
import contextlib
import numpy as np
import concourse.bass as bass
import concourse.mybir as mybir
from concourse.bass_utils import run_bass_kernel_spmd

F32 = mybir.dt.float32
BF16 = mybir.dt.bfloat16
AF = mybir.ActivationFunctionType
ALU = mybir.AluOpType
AX = mybir.AxisListType

ENGS = ("pe", "act", "dve", "pool", "sp")

D = 1024
NIN = 7688
DFF = 2816
NFC = 22
EPS = 1e-6
NMEM = 256
C_HQ, C_HF, C_HI, C_HG, C_MQ, C_MK, C_MV, C_MO, C_MI, C_MF, C_XQ, C_GL = (
    0, 512, 1024, 1536, 2048, 2560, 3072, 3584, 4096, 4100, 4104, 4616)


class Sched:
    LAT = 150.0

    def __init__(self, nc):
        self.nc = nc
        self.recs = []
        self.lastw = {}
        self.readers = {}
        self.last_dma = {}
        self.dma_cnt = {}
        self.final_waits = {}
        self.cnt = {e: 0 for e in ENGS}
        self.reorder = True
        self.trace_cp = False
        self.prio = 'bl'
        self.cause = {}
        self.start = {}

    def _deps(self, eng, reads, writes):
        deps = set()
        for k in reads:
            w = self.lastw.get(k)
            if w is not None:
                deps.add(w)
            if k.startswith("ps"):
                for t in self.readers.get(k, ()):
                    if self.recs[t]["eng"] != eng:
                        deps.add(t)
        for k in writes:
            w = self.lastw.get(k)
            if w is not None:
                deps.add(w)
            for t in self.readers.get(k, ()):
                deps.add(t)
        return deps

    def _commit(self, oid, reads, writes):
        for k in reads:
            self.readers.setdefault(k, []).append(oid)
        for k in writes:
            self.lastw[k] = oid
            self.readers[k] = []

    def op(self, eng, fn, reads=(), writes=(), cost=300.0):
        oid = len(self.recs)
        self.recs.append(dict(eng=eng, fn=fn, deps=self._deps(eng, reads, writes), cost=cost, dma=None))
        self.cnt[eng] += 1
        self._commit(oid, reads, writes)
        return oid

    def dma(self, eng, fn, semkey, reads=(), writes=(), final=False, cost=4000.0):
        oid = len(self.recs)
        deps = self._deps(eng, reads, writes)
        if semkey in self.last_dma:
            deps.add(self.last_dma[semkey])
        v = self.dma_cnt.get(semkey, 0) + 16
        self.dma_cnt[semkey] = v
        self.last_dma[semkey] = oid
        self.recs.append(dict(eng=eng, fn=fn, deps=deps, cost=cost, dma=(semkey, v)))
        self.cnt[eng] += 1
        self._commit(oid, reads, writes)
        if final:
            self.final_waits[semkey] = v
        return oid

    def schedule(self):
        import heapq
        recs = self.recs
        n = len(recs)
        if not self.reorder:
            order = {e: [] for e in ENGS}
            for i, r in enumerate(recs):
                order[r["eng"]].append(i)
            return order
        succ = [[] for _ in range(n)]
        ndep = [0] * n
        for i, r in enumerate(recs):
            ndep[i] = len(r["deps"])
            for d in r["deps"]:
                succ[d].append(i)
        bl = [0.0] * n
        for i in range(n - 1, -1, -1):
            m = 0.0
            for j in succ[i]:
                v = bl[j] + self.LAT
                if v > m:
                    m = v
            bl[i] = recs[i]["cost"] + m
        ready_t = [0.0] * n
        fin = [0.0] * n
        heaps = {e: [] for e in ENGS}
        for i in range(n):
            if ndep[i] == 0:
                heapq.heappush(heaps[recs[i]["eng"]], (0.0, i))
        free = {e: 0.0 for e in ENGS}
        order = {e: [] for e in ENGS}
        done = 0
        while done < n:
            best = None
            for e in ENGS:
                h = heaps[e]
                if not h:
                    continue
                st = max(free[e], h[0][0])
                if best is None or st < best[0]:
                    best = (st, e)
            st, e = best
            h = heaps[e]
            cand = []
            while h and h[0][0] <= st:
                cand.append(heapq.heappop(h))
            pick = max(cand, key=lambda c: (bl[c[1]], -c[1])) if self.prio == 'bl' else min(cand, key=lambda c: c[1])
            for c in cand:
                if c is not pick:
                    heapq.heappush(h, c)
            i = pick[1]
            r = recs[i]
            if self.trace_cp:
                dmax, darg = -1.0, None
                for d in r["deps"]:
                    if fin[d] + self.LAT > dmax:
                        dmax, darg = fin[d] + self.LAT, d
                if order[e] and free[e] >= dmax:
                    self.cause[i] = ("eng", order[e][-1])
                else:
                    self.cause[i] = ("dep", darg)
                self.start[i] = st
            if r["dma"] is None:
                fin[i] = st + r["cost"]
                free[e] = fin[i]
            elif e == "pool":
                fin[i] = st + r["cost"]
                free[e] = st + 1200.0
            else:
                fin[i] = st + r["cost"]
                free[e] = st + 60.0
            order[e].append(i)
            done += 1
            for j in succ[i]:
                rt = fin[i] + self.LAT
                if rt > ready_t[j]:
                    ready_t[j] = rt
                ndep[j] -= 1
                if ndep[j] == 0:
                    heapq.heappush(heaps[recs[j]["eng"]], (ready_t[j], j))
        self.sim_time = max(fin) if n else 0.0
        self.fin = fin
        return order

    def emit(self, es, final_engine="sp"):
        nc = self.nc
        recs = self.recs
        order = self.schedule()
        tok = [None] * len(recs)
        for e in ENGS:
            c = 0
            for i in order[e]:
                r = recs[i]
                if r["dma"] is not None:
                    tok[i] = r["dma"]
                else:
                    c += 1
                    tok[i] = (e, c)
        sems = {}
        for e in ENGS:
            sems[e] = es.enter_context(nc.semaphore("s_" + e))
        for i, sk in enumerate(self.dma_cnt):
            sems[sk] = es.enter_context(nc.semaphore("d%d" % i))
        block = es.enter_context(nc.Block())
        fw = list(self.final_waits.items())

        def run(eng_name):
            def body(eng):
                known = {}
                for i in order[eng_name]:
                    r = recs[i]
                    need = {}
                    for d in r["deps"]:
                        sk, v = tok[d]
                        if eng_name == "pe" and sk == "pe":
                            continue
                        if need.get(sk, 0) < v:
                            need[sk] = v
                    for sk, v in need.items():
                        if known.get(sk, 0) >= v:
                            continue
                        known[sk] = v
                        eng.wait_ge(sems[sk], v)
                    ins = r["fn"](eng)
                    if r["dma"] is None:
                        ins.then_inc(sems[eng_name], 1)
                    else:
                        ins.then_inc(sems[r["dma"][0]], 16)
                if eng_name == final_engine:
                    for wk, wv in fw:
                        eng.wait_ge(sems[wk], wv)
            return body

        block.tensor(run("pe"))
        block.scalar(run("act"))
        block.vector(run("dve"))
        block.gpsimd(run("pool"))
        block.sync(run("sp"))


class Builder:
    def __init__(self, TT=256, n_prompt=2, T_prompt=4096, T_sample=32):
        self.TT = TT
        self.n_prompt = n_prompt
        self.T_prompt = T_prompt
        self.T_sample = T_sample
        self.nc = bass.Bass("TRN2", target_bir_lowering=False)
        self.es = contextlib.ExitStack()
        self.S = Sched(self.nc)
        self.uid = 0
        self.ps_free = list(range(8))
        self.ring_i = 0
        self.slot_i = 0
        self.piece_in_slot = {}

    def ck(self, name):
        import os
        if os.environ.get("STOP_AT") == name:
            raise StopIteration(name)

    def sb(self, name, shape, dt=F32):
        return self.es.enter_context(self.nc.sbuf_tensor(name, list(shape), dt))

    def dram(self, name, shape, dt=F32, kind="ExternalInput"):
        return self.nc.dram_tensor(name, list(shape), dt, kind=kind).ap()

    @staticmethod
    def _n(ap):
        n = 1
        for d in ap.shape[1:]:
            n *= int(d)
        return n

    def _ec(self, eng, ap):
        n = self._n(ap)
        if eng == "act":
            return 230.0 + 0.85 * n
        if eng == "pool":
            return 150.0 + 1.6 * n
        return 70.0 + 1.05 * n

    def act(self, out, in_, func, r, w, bias=None, scale=None, accum=None):
        kw = {}
        if bias is not None:
            kw["bias"] = bias
        if scale is not None:
            kw["scale"] = scale
        if accum is not None:
            kw["accum_out"] = accum
        self.S.op("act", lambda e: e.activation(out=out, in_=in_, func=func, **kw), r, w, cost=self._ec("act", out) + (100 if accum is not None else 0))

    def ts(self, eng, out, in0, s1, s2, op0, op1, r, w):
        c = self._ec(eng, out)
        if op1 is None:
            self.S.op(eng, lambda e: e.tensor_scalar(out=out, in0=in0, scalar1=s1, scalar2=None, op0=op0), r, w, cost=c)
        else:
            self.S.op(eng, lambda e: e.tensor_scalar(out=out, in0=in0, scalar1=s1, scalar2=s2, op0=op0, op1=op1), r, w, cost=c)

    def stt(self, out, in0, scalar, in1, op0, op1, r, w):
        self.S.op("dve", lambda e: e.scalar_tensor_tensor(out=out, in0=in0, scalar=scalar, in1=in1, op0=op0, op1=op1), r, w,
                  cost=self._ec("dve", out))

    def tt(self, eng, out, in0, in1, op, r, w):
        self.S.op(eng, lambda e: e.tensor_tensor(out=out, in0=in0, in1=in1, op=op), r, w, cost=self._ec(eng, out))

    def cp(self, eng, out, in_, r, w):
        if eng == "act":
            self.S.op("act", lambda e: e.activation(out=out, in_=in_, func=AF.Copy), r, w, cost=self._ec("act", out))
        else:
            self.S.op(eng, lambda e: e.tensor_copy(out=out, in_=in_), r, w, cost=self._ec(eng, out))

    def mm(self, out, lhsT, rhs, start, stop, r, w):
        n = self._n(rhs)
        c = 60.0 + 0.33 * max(n, 64)
        if rhs.dtype == F32:
            c *= 4
        self.S.op("pe", lambda e: e.matmul(out, lhsT=lhsT, rhs=rhs, start=start, stop=stop), r, w, cost=c)

    def tr(self, out, in_, ident, r, w):
        self.S.op("pe", lambda e: e.matmul(out, lhsT=in_, rhs=ident, start=True, stop=True, is_transpose=True), r, w, cost=170.0)

    def memset(self, eng, ap, val, r, w):
        self.S.op(eng, lambda e: e.memset(ap, val), r, w, cost=self._ec(eng, ap))

    def dma(self, out, in_, semkey, r, w, eng="sp", final=False, slow=False):
        nbytes = self._n(out) * int(out.shape[0]) * (2 if out.dtype == BF16 else 4)
        c = 2000.0 + nbytes * 0.005
        if eng == "pool":
            c = 3000.0 + nbytes * 0.018
        if slow:
            self.S.dma(eng, lambda e: e.dma_start(out=out, in_=in_, allow_slow_non_contiguous=True), semkey, r, w, final, cost=c)
        else:
            self.S.dma(eng, lambda e: e.dma_start(out=out, in_=in_), semkey, r, w, final, cost=c)

    def pow_m05(self, out, in_, r, w):
        n = in_.shape[-1]
        P = in_.shape[0]
        self.S.op("pool", lambda e: e.tensor_tensor(out=out, in0=in_, in1=self.mhalf[0:P, 0:n], op=ALU.pow), list(r) + ["consts"], w)

    def psum(self):
        if not self.ps_free:
            raise RuntimeError("out of PSUM banks")
        i = self.ps_free.pop(0)
        return self.ps[i], "ps%d" % i

    def pfree(self, pk):
        i = int(pk[2:])
        assert i not in self.ps_free
        self.ps_free.append(i)

    def load_T(self, dst, src, n, r, w, semkey):
        self.dma(self.stg[0:n, :], src, semkey, [], ["stg"])
        pt, pk = self.psum()
        self.tr(pt[:, 0:n], self.stg[0:n, :], self.ident_f[0:n, 0:n], ["stg", "consts"], [pk])
        self.cp("dve", dst, pt[:, 0:n], [pk] + list(r), w)
        self.pfree(pk)

    def store_T(self, dst, src, n, r, semkey):
        pt, pk = self.psum()
        self.tr(pt[0:n, 0:128], src, self.ident_f[:], list(r) + ["consts"], [pk])
        self.cp("dve", self.stg[0:n, :], pt[0:n, 0:128], [pk], ["stg"])
        self.pfree(pk)
        self.dma(dst, self.stg[0:n, :], semkey, ["stg"], [], final=True)

    def tmp(self):
        i = self.ring_i
        self.ring_i = (i + 1) % self.NRING
        return self.ringbuf[:, i, :], "ring%d" % i

    def tmp4(self):
        i = 0 if self.ring_i in (0, 5, 6, 7) else 4
        if self.ring_i in (1, 2, 3, 4):
            i = 4
        self.ring_i = (i + 4) % self.NRING
        return self.ringbuf[:, i:i + 4, :].rearrange("p a b -> p (a b)"), ["ring%d" % (i + k) for k in range(4)]

    def tmp2(self):
        if self.ring_i == self.NRING - 1:
            self.ring_i = 0
        i = self.ring_i
        self.ring_i = (i + 2) % self.NRING
        return self.ringbuf[:, i:i + 2, :].rearrange("p a b -> p (a b)"), ["ring%d" % i, "ring%d" % (i + 1)]

    def define_pieces(self):
        P = []
        self.piece_gain = {}
        w_in = self.w_in
        for i in range(2):
            P.append(("mkv%d" % i, self.w_mem_kv[:, 512 * i:512 * (i + 1)], 8, 512))
            self.piece_gain["mkv%d" % i] = 2
        for nm, c0 in (("hq", C_HQ), ("hf", C_HF), ("hi", C_HI), ("hg", C_HG), ("mq", C_MQ), ("mk", C_MK),
                       ("mv", C_MV), ("mo", C_MO), ("xq", C_XQ)):
            P.append((nm, w_in[:, c0:c0 + 512], 8, 512))
            self.piece_gain[nm] = 0
        for br in range(3):
            P.append(("br%d" % br, self.w_branch[br], 4, 1024))
            for half in range(2):
                i = 2 * br + half
                P.append(("gl%d" % i, w_in[:, C_GL + 512 * i:C_GL + 512 * (i + 1)], 8, 512))
                self.piece_gain["gl%d" % i] = 0
        for i in range(2):
            P.append(("out%d" % i, self.w_out[:, 512 * i:512 * (i + 1)], 8, 512))
        for i in range(11):
            P.append(("up%d" % i, self.w_up[:, 512 * i:512 * (i + 1)], 8, 512))
            self.piece_gain["up%d" % i] = 1
        for g in range(4):
            for kh in range(2):
                P.append(("dn%d_%d" % (g, kh), self.w_down[kh * 1408:(kh + 1) * 1408, 256 * g:256 * (g + 1)], 11, 256))
        self.pieces = {p[0]: (idx,) + p[1:] for idx, p in enumerate(P)}
        self.wsc = self.dram("wscratch", [len(P), 128, 4096], BF16, kind="Internal")

    def slot_view(self, s, kc, n):
        return self.wslots[s][:, 0:kc * n].rearrange("p (k n) -> p k n", k=kc)

    def convert_weights(self):
        self.conv_names = list(self.pieces.keys())
        self.conv_i = 0

    def convert_next(self):
        ci = self.conv_i
        nm = self.conv_names[ci]
        self.conv_i += 1
        idx, src, kc, n = self.pieces[nm]
        stg, keys = self.tmp4()
        flat = stg.bitcast(BF16)[:, 0:kc * n]
        view = flat.rearrange("p (k n) -> p k n", k=kc)
        self.dma(view, src.rearrange("(k p) n -> p k n", p=128), "wcv%d" % (ci % 2), [], keys, eng="pool")
        if nm in self.piece_gain:
            gi_ = self.piece_gain[nm]
            for k_ in range(kc):
                if k_ % 2 == 0:
                    self.ts("dve", view[:, k_, :], view[:, k_, :], self.g_fm[:, gi_, k_:k_ + 1], None, ALU.mult, None, keys + ["g_fm"], keys)
                else:
                    self.act(view[:, k_, :], view[:, k_, :], AF.Copy, keys + ["g_fm"], keys, scale=self.g_fm[:, gi_, k_:k_ + 1])
        self.dma(self.wsc[idx, :, 0:kc * n], flat, "wst%d" % (ci % 2), keys, ["wsc%d" % idx])

    def load_piece(self, nm):
        idx, src, kc, n = self.pieces[nm]
        while self.conv_i < len(self.conv_names) and self.conv_i <= idx + 3:
            self.convert_next()
        s = self.slot_i
        self.slot_i = (s + 1) % len(self.wslots)
        key = "wslot%d" % s
        self.dma(self.wslots[s][:, 0:kc * n], self.wsc[idx, :, 0:kc * n], "wsl%d" % s, ["wsc%d" % idx], [key])
        return self.slot_view(s, kc, n), key

    def build(self):
        nc = self.nc
        TT = self.TT
        npr = self.n_prompt
        Tp = self.T_prompt
        Ts = self.T_sample
        self.x_prompt = self.dram("x_prompt", [npr, Tp, D])
        self.x_sample = self.dram("x_sample", [1, Ts, D])
        self.state_hgrn = self.dram("state_hgrn", [1, 4, 128, 128])
        self.state_C = self.dram("state_mlstm_C", [1, 4, 128, 128])
        self.state_n = self.dram("state_mlstm_n", [1, 4, 128])
        self.state_m = self.dram("state_mlstm_m", [1, 4])
        self.state_conv = self.dram("state_ffn_conv", [1, 2, DFF])
        self.cache_k = self.dram("cache_mem_k", [1, NMEM, 512])
        self.cache_v = self.dram("cache_mem_v", [1, NMEM, 512])
        self.mem_prompt = self.dram("mem_prompt", [npr, NMEM, D])
        self.norm1 = self.dram("norm1", [1, D])
        self.w_in = self.dram("w_in", [D, NIN])
        self.b_in = self.dram("b_in", [1, NIN])
        self.fgb = self.dram("ml_fgate_bias", [1, 4])
        self.lbl = self.dram("hg_lb_logits", [2, 512])
        self.hg_norm = self.dram("hg_norm", [1, 512])
        self.ml_norm = self.dram("ml_norm", [1, 512])
        self.mem_norm = self.dram("mem_norm", [1, D])
        self.w_mem_kv = self.dram("w_mem_kv", [D, D])
        self.w_branch = self.dram("w_branch", [3, 512, D])
        self.w_out = self.dram("w_out", [D, D])
        self.norm2 = self.dram("norm2", [1, D])
        self.w_up = self.dram("w_up", [D, 2 * DFF])
        self.conv_w = self.dram("ffn_conv_w", [3, DFF])
        self.conv_b = self.dram("ffn_conv_b", [1, DFF])
        self.w_down = self.dram("w_down", [DFF, D])
        self.final_norm = self.dram("final_norm", [1, D])
        o = "ExternalOutput"
        self.y_prompt = self.dram("y_prompt", [npr, Tp, D], kind=o)
        self.y_sample = self.dram("y_sample", [1, Ts, D], kind=o)
        self.o_hgrn_p = self.dram("hgrn_p", [npr, 4, 128, 128], kind=o)
        self.o_C_p = self.dram("mlstm_C_p", [npr, 4, 128, 128], kind=o)
        self.o_n_p = self.dram("mlstm_n_p", [npr, 4, 128], kind=o)
        self.o_m_p = self.dram("mlstm_m_p", [npr, 4], kind=o)
        self.o_mk_p = self.dram("mem_k_p", [npr, NMEM, 512], kind=o)
        self.o_mv_p = self.dram("mem_v_p", [npr, NMEM, 512], kind=o)
        self.o_cv_p = self.dram("ffn_conv_p", [npr, 2, DFF], kind=o)
        self.o_hgrn_s = self.dram("hgrn_s", [1, 4, 128, 128], kind=o)
        self.o_C_s = self.dram("mlstm_C_s", [1, 4, 128, 128], kind=o)
        self.o_n_s = self.dram("mlstm_n_s", [1, 4, 128], kind=o)
        self.o_m_s = self.dram("mlstm_m_s", [1, 4], kind=o)
        self.o_cv_s = self.dram("ffn_conv_s", [1, 2, DFF], kind=o)

        self.ps = [self.es.enter_context(nc.psum_tensor("ps%d" % i, [128, 512], F32)) for i in range(8)]
        RW = TT + 2
        self.NRING = 8
        self.RW = max(RW, 514)
        self.ringbuf = self.sb("ringbuf", [128, self.NRING, self.RW])
        self.wslots = [self.sb("wslot%d" % i, [128, 4096], BF16) for i in range(4)]
        NS = TT // 128
        self.xt = self.sb("xt", [128, NS, D])
        self.u_tm = self.sb("u_tm", [128, D], BF16)
        self.u_fm = self.sb("u_fm", [128, 8, TT], BF16)
        self.gb = self.sb("gb", [128, D])
        self.w_if = self.sb("w_if", [128, 8, 8], BF16)
        self.mhalf = self.sb("mhalf", [128, 16])
        self.one_col = self.sb("one_col", [128, 1])
        self.stg = self.sb("stg", [128, 128])
        self.carryT = self.sb("carryT", [128, 2 * NFC])
        self.Ssnap = [self.sb("Ssnap%d" % c, [128, 4, 128], BF16) for c in range(2)]
        self._rows_started = False
        self._prevTT = 0
        self.ident_f = self.sb("ident_f", [128, 128])
        self.ident_b = self.sb("ident_b", [128, 128], BF16)
        self.maskH = self.sb("maskH", [128, 128])
        self.maskM = self.sb("maskM", [128, 128])
        self.sel = self.sb("sel", [4, 4, 128])
        self.rmask = self.sb("rmask", [128, 512], BF16)
        self.ones_bf = self.sb("ones_bf", [128, 128], BF16)
        self.ones4 = self.sb("ones4", [4, 512], BF16)
        self.bfm_a = self.sb("bfm_a", [128, 32])
        self.bfm_x = self.sb("bfm_x", [128, 4])
        self.bfm_g = self.sb("bfm_g", [128, 24])
        self.bq_half = self.sb("bq_half", [128, 4])
        self.bf_neg = self.sb("bf_neg", [128, 4])
        self.bk_s = self.sb("bk_s", [128, 4])
        self.bg_half = self.sb("bg_half", [128, 24])
        self.brow_b = self.sb("brow_b", [1, 4, 512], BF16)
        self.bi = self.sb("bi", [4, 1])
        self.bfn = self.sb("bfn", [4, 2])
        self.lt = self.sb("lt", [128, 2, 4])
        self.lb = self.sb("lb", [128, 4])
        self.hgn = self.sb("hgn", [128, 512], BF16)
        self.mln = self.sb("mln", [128, 512], BF16)
        self.cw = self.sb("cw", [128, 3, NFC])
        self.cb = self.sb("cb", [128, NFC])
        self.qt = self.sb("qt", [128, 4, TT], BF16)
        self.kt = self.sb("kt", [128, 4, TT], BF16)
        self.kp = self.sb("kp", [128, 4, 128], BF16)
        self.EA = self.sb("EA", [128, 4, TT], BF16)
        self.dec = self.sb("dec", [128, 4, 16])
        self.v_tm = self.sb("v_tm", [128, NS, 512], BF16)
        self.gate_h = self.sb("gate_h", [128, NS, 512], BF16)
        self.kpT = self.sb("kpT", [128, 512], BF16)
        self.STh = self.sb("STh", [128, 128], BF16)
        self.S_f = self.sb("S_f", [128, 4, 128])
        self.S_b = self.sb("S_b", [128, 4, 128], BF16)
        self.oh_tm = self.sb("oh_tm", [128, 512], BF16)
        self.o_h_fm = self.sb("o_h_fm", [128, 4, TT], BF16)
        self.ssq = self.sb("ssq", [128, 16])
        self.g_fm = self.sb("g_fm", [128, 3, 8])
        self.ssq_h = self.sb("ssq_h", [128, 16])
        self.ssq_m = self.sb("ssq_m", [128, 8])
        self.ssq_a = self.sb("ssq_a", [128, 8])
        self.dsa = self.sb("dsa", [128, 16])
        self.mq = self.sb("mq", [128, 4, TT], BF16)
        self.mk = self.sb("mk", [128, 4, TT], BF16)
        self.vm_tm = self.sb("vm_tm", [128, NS, 4, 132], BF16)
        self.gate_m = self.sb("gate_m", [128, NS, 512], BF16)
        self.Bx = self.sb("Bx", [4, TT + 1])
        self.Gx = self.sb("Gx", [4, TT + 1])
        self.rows = self.sb("rows", [4, 4, TT])
        self.m_last = self.sb("m_last", [4, 1])
        self.sc_tm = self.sb("sc_tm", [128, 16])
        self.Wm = self.sb("Wm", [128, 4, 128])
        self.STm = self.sb("STm", [128, 4, 128], BF16)
        self.kw_tm = self.sb("kw_tm", [128, 4, 128], BF16)
        self.numI = self.sb("numI", [128, 512])
        self.num = self.sb("num", [128, 512])
        self.dsm = self.sb("dsm", [128, 32])
        self.decm = self.sb("decm", [128, 4])
        self.C_f = self.sb("C_f", [128, 4, 128])
        self.C_b = self.sb("C_b", [128, 4, 128], BF16)
        self.n_f = self.sb("n_f", [128, 4])
        self.n_b = self.sb("n_b", [128, 4], BF16)
        self.hm_tm = self.sb("hm_tm", [128, 512], BF16)
        self.h_m_fm = self.sb("h_m_fm", [128, 4, TT], BF16)
        self.xq = self.sb("xq", [128, 4, TT], BF16)
        self.KT = self.sb("KT", [128, 4, NMEM], BF16)
        self.Vb = self.sb("Vb", [128, 2, 512], BF16)
        self.pex = self.sb("pex", [128, 4, NMEM], BF16)
        self.pT = self.sb("pT", [128, 8, 128], BF16)
        self.o_x_fm = self.sb("o_x_fm", [128, 4, TT], BF16)
        self.macc = self.sb("macc", [128, 8, max(TT, 512)], BF16)
        self.merged = self.sb("merged", [128, 8, TT], BF16)
        self.kvf = self.gb[:].rearrange("p (a b) -> p a b", a=2)
        self.memn_fm = self.merged[:, :, 0:NMEM]
        self.carry = self.sb("carry", [128, NFC, 2])
        self.Gxb = [self.sb("Gxb%d" % i, [128, TT + 2]) for i in range(2)]
        self.gxi = 0

        try:
            self._program()
        except StopIteration as ex:
            print("STOPPED AT", ex)
        self.S.emit(self.es)
        return nc

    def _program(self):
        TT = self.TT
        npr, Tp, Ts = self.n_prompt, self.T_prompt, self.T_sample
        self.define_pieces()
        self.act_ch = []
        for tl, key in ((self.o_h_fm, "o_h_fm"), (self.h_m_fm, "h_m_fm"), (self.o_x_fm, "o_x_fm")):
            for i in range(4):
                self.act_ch.append((tl[:, i, :], key))
        for i in range(8):
            self.act_ch.append((self.merged[:, i, :], "merged"))
        for i in range(2):
            self.act_ch.append((self.macc[:, i, 0:TT], "macc%d" % i))
        self.setup_consts()
        self.ck("consts")
        self.convert_weights()
        self.ck("conv")

        seqs = []
        for b in range(npr):
            seqs.append(dict(kind="p", idx=b, T=Tp, TT=TT, P=128, L=64,
                             x=self.x_prompt[b], y=self.y_prompt[b]))
        seqs.append(dict(kind="s", idx=0, T=Ts, TT=Ts, P=Ts, L=Ts,
                         x=self.x_sample[0], y=self.y_sample[0]))
        for sq in seqs:
            self.seq_setup(sq)
            self.load_final_gain()
            self.ck("setup")
            for ti in range(sq["T"] // sq["TT"]):
                self.tile(sq, ti)
                self.ck("tile")
            self.seq_finish(sq)
            self.ck("seq")

    def setup_consts(self):
        K = ["consts"]
        S = self.S
        self.memset("pool", self.mhalf[:], -0.5, [], K)
        self.memset("pool", self.one_col[:], 1.0, [], K)
        self.memset("pool", self.ident_f[:], 0.0, [], K)
        S.op("pool", lambda e: e.affine_select(out=self.ident_f[:], in_=self.ident_f[:], pattern=[[-1, 128]],
                                               compare_op=ALU.not_equal, fill=1.0, base=0, channel_multiplier=1), K, K)
        self.cp("dve", self.ident_b[:], self.ident_f[:], K, K)
        self.memset("pool", self.maskH[:], 1.0, K, K)
        S.op("pool", lambda e: e.affine_select(out=self.maskH[:], in_=self.maskH[:], pattern=[[1, 128]],
                                               compare_op=ALU.is_ge, fill=0.0, base=0, channel_multiplier=-1), K, K)
        self.memset("pool", self.maskH[0:64, 64:128], 0.0, K, K)
        self.memset("pool", self.maskM[:], 0.0, K, K)
        S.op("pool", lambda e: e.affine_select(out=self.maskM[:], in_=self.maskM[:], pattern=[[1, 128]],
                                               compare_op=ALU.is_ge, fill=-1e30, base=0, channel_multiplier=-1), K, K)
        self.memset("pool", self.sel[:], 0.0, K, K)
        S.op("pool", lambda e: e.affine_select(out=self.sel[:], in_=self.sel[:], pattern=[[-1, 4], [0, 128]],
                                               compare_op=ALU.not_equal, fill=1.0, base=0, channel_multiplier=1), K, K)
        self.memset("pool", self.rmask[:], 1.0, K, K)
        self.memset("pool", self.rmask[:].rearrange("p (c l) -> p c l", l=64)[:, :, 0:1], 0.0, K, K)
        self.memset("pool", self.ones_bf[:], 1.0, K, K)
        self.memset("pool", self.ones4[:], 1.0, K, K)
        self.memset("pool", self.vm_tm[:], 1.0, K, ["vm_tm"])
        for gi_, gsrc in enumerate((self.norm1, self.norm2, self.mem_norm)):
            self.load_T(self.g_fm[:, gi_, :], gsrc[0].rearrange("(j p) -> j p", p=128), 8, [], ["g_fm"], "c0")
        b = self.b_in[0]
        self.load_T(self.bfm_a[:], b[0:4096].rearrange("(j p) -> j p", p=128), 32, [], ["bfm"], "c0")
        self.load_T(self.bfm_x[:], b[C_XQ:C_XQ + 512].rearrange("(j p) -> j p", p=128), 4, [], ["bfm"], "c0")
        self.load_T(self.bfm_g[:], b[C_GL:C_GL + 3072].rearrange("(j p) -> j p", p=128), 24, [], ["bfm"], "c0")
        for gi_, c0_ in enumerate((C_HI, C_HG, C_MV, C_MO)):
            rt, rk = self.tmp()
            self.dma(rt[0:1, 0:512], self.b_in[0:1, c0_:c0_ + 512], "c3", [], [rk])
            self.cp("dve", self.brow_b[0:1, gi_, :], rt[0:1, 0:512], [rk], K)
        self.dma(self.bi[:], self.b_in[0, C_MI:C_MI + 4].rearrange("(p o) -> p o", o=1), "c4", [], ["bi"], slow=True)
        self.dma(self.bfn[:, 0:1], self.b_in[0, C_MF:C_MF + 4].rearrange("(p o) -> p o", o=1), "c5", [], ["bfn"], slow=True)
        self.dma(self.bfn[:, 1:2], self.fgb[0].rearrange("(p o) -> p o", o=1), "c6", [], ["bfn"], slow=True)
        self.load_T(self.lt[:].rearrange("p r h -> p (r h)"), self.lbl.rearrange("r (h p) -> (r h) p", p=128), 8, [], ["lt"], "c0")
        rt, rk = self.tmp()
        self.dma(rt[:, 0:512], self.hg_norm[0].partition_broadcast(128), "c8", [], [rk])
        self.ts("dve", self.hgn[:], rt[:, 0:512], 0.5, None, ALU.mult, None, [rk], ["hgn"])
        rt, rk = self.tmp()
        self.dma(rt[:, 0:512], self.ml_norm[0].partition_broadcast(128), "c9", [], [rk])
        self.ts("dve", self.mln[:], rt[:, 0:512], 0.5, None, ALU.mult, None, [rk], ["mln"])
        self.load_T(self.cw[:].rearrange("p r f -> p (r f)"), self.conv_w.rearrange("r (f p) -> (r f) p", p=128), 3 * NFC, [], ["cw"], "c0")
        self.load_T(self.cb[:], self.conv_b[0].rearrange("(f p) -> f p", p=128), NFC, [], ["cw"], "c0")
        self.dma(self.w_if[:], self.w_in[:, C_MI:C_MI + 8].rearrange("(k p) n -> p k n", p=128), "c12", [], ["w_if"], eng="pool")
        for k_ in range(8):
            self.ts("dve", self.w_if[:, k_, :], self.w_if[:, k_, :], self.g_fm[:, 0, k_:k_ + 1], None, ALU.mult, None, ["w_if", "g_fm"], ["w_if"])
        self.ts("dve", self.bq_half[:], self.bfm_a[:, 0:4], 0.5, None, ALU.mult, None, ["bfm"], K)
        self.ts("dve", self.bf_neg[:], self.bfm_a[:, 4:8], -1.0, None, ALU.mult, None, ["bfm"], K)
        self.ts("dve", self.bk_s[:], self.bfm_a[:, 20:24], 128 ** -0.5, None, ALU.mult, None, ["bfm"], K)
        self.ts("dve", self.bg_half[:], self.bfm_g[:], 0.5, None, ALU.mult, None, ["bfm"], K)
        self.stt(self.bfn[:, 0:1], self.bfn[:, 0:1], -1.0, self.bfn[:, 1:2], ALU.mult, ALU.subtract, ["bfn"], ["bfn"])
        self.tt("dve", self.lb[:], self.lt[:, 1, :], self.lt[:, 0, :], ALU.subtract, ["lt"], ["lb"])
        self.act(self.lb[:], self.lb[:], AF.Exp, ["lb"], ["lb"])
        self.ts("dve", self.lb[:], self.lb[:], 1.0, None, ALU.add, None, ["lb"], ["lb"])
        self.S.op("dve", lambda e: e.reciprocal(out=self.lb[:], in_=self.lb[:]), ["lb"], ["lb"])

    def rstd_rows(self, src, Pn, src_keys, site=0):
        c = 4 * site
        sk = "ssq%d" % site
        jt, jk = self.tmp()
        self.act(jt[0:Pn, 0:512].bitcast(BF16), src, AF.Square, src_keys, [sk, jk], accum=self.ssq[0:Pn, c:c + 1])
        self.ts("dve", self.ssq[0:Pn, c + 1:c + 2], self.ssq[0:Pn, c:c + 1], 1.0 / D, EPS, ALU.mult, ALU.add, [sk], [sk])
        self.pow_m05(self.ssq[0:Pn, c + 2:c + 3], self.ssq[0:Pn, c + 1:c + 2], [sk], [sk])
        return self.ssq[0:Pn, c + 2:c + 3], sk

    def norm_transpose(self, src_fn, Pn, nsub, site, dst, dst_key, src_keys):
        for j in range(nsub):
            if src_keys == "provider":
                src, skeys = src_fn(j)
            else:
                src = src_fn(j)
                skeys = ["xt%d" % j] if src_keys is None else src_keys
            rs, sk = self.rstd_rows(src, Pn, skeys, site)
            self.ts("dve", self.u_tm[0:Pn, :], src, rs, None, ALU.mult, None, list(skeys) + [sk], ["u_tm"])
            pt, pk = self.psum()
            ptb = pt[:].bitcast(BF16)
            for kc in range(8):
                self.tr(ptb[:, kc * Pn:(kc + 1) * Pn], self.u_tm[0:Pn, kc * 128:(kc + 1) * 128], self.ident_b[0:Pn, 0:Pn],
                        ["u_tm", "consts"], [pk])
            self.cp("dve", dst[:, :, j * Pn:(j + 1) * Pn], ptb[:, 0:8 * Pn].rearrange("p (k t) -> p k t", k=8), [pk], [dst_key])
            self.pfree(pk)

    def seq_setup(self, sq):
        self._rows_started = False
        SK = ["S_f%d" % h for h in range(4)]
        SBK = ["S_b%d" % h for h in range(4)]
        if sq["kind"] == "p":
            b = sq["idx"]
            self.memset("pool", self.S_f[:], 0.0, [], SK)
            self.memset("pool", self.S_b[:], 0.0, [], SBK)
            self.memset("pool", self.C_f[:], 0.0, [], ["C_f"])
            self.memset("pool", self.C_b[:], 0.0, [], ["C_b"])
            self.memset("pool", self.n_f[:], 0.0, [], ["n_f"])
            self.memset("pool", self.n_b[:], 0.0, [], ["n_b"])
            self.memset("pool", self.carry[:], 0.0, [], ["carry"])
            self.memset("pool", self.Bx[:, 0:1], 0.0, [], ["Bx"])
            self.memset("pool", self.Gx[:, 0:1], 0.0, [], ["Gx"])
            memt = self.xt
            self.dma(memt[:, 0:2, :], self.mem_prompt[b].rearrange("(j p) d -> p j d", p=128), "xt0", [], ["xt0", "xt1"])
            self.ck("s_pre")
            self.norm_transpose(lambda j: memt[:, j, :], 128, 2, 3, self.memn_fm, "merged", None)
            self.ck("s_nt")
            wk, wkk = self.load_piece("mkv0")
            for h in range(4):
                pt, pk = self.psum()
                for kc in range(8):
                    self.mm(pt[:, 0:NMEM], wk[:, kc, h * 128:(h + 1) * 128], self.memn_fm[:, kc, :], kc == 0, kc == 7,
                            [wkk, "merged"], [pk])
                self.cp("act", self.KT[:, h, :], pt[:, 0:NMEM], [pk], ["KT"])
                self.pfree(pk)
            for mc in range(2):
                pt, pk = self.psum()
                for kc in range(8):
                    self.mm(pt[:, :], self.memn_fm[:, kc, mc * 128:(mc + 1) * 128], wk[:, kc, :], kc == 0, kc == 7,
                            [wkk, "merged"], [pk])
                self.cp("act", self.kvf[:, mc, :], pt[:, :], [pk], ["gb"])
                self.pfree(pk)
            self.ck("s_kt")
            self.dma(self.o_mk_p[b].rearrange("(j p) d -> p j d", p=128), self.kvf[:], "kvo", ["gb"], [], final=True)
            self.ck("s_ko")
            wv, wvk = self.load_piece("mkv1")
            for mc in range(2):
                pt, pk = self.psum()
                for kc in range(8):
                    self.mm(pt[:, :], self.memn_fm[:, kc, mc * 128:(mc + 1) * 128], wv[:, kc, :], kc == 0, kc == 7,
                            [wvk, "merged"], [pk])
                self.cp("act", self.kvf[:, mc, :], pt[:, :], [pk], ["gb"])
                self.cp("dve", self.Vb[:, mc, :], pt[:, :], [pk], ["Vb"])
                self.pfree(pk)
            self.dma(self.o_mv_p[b].rearrange("(j p) d -> p j d", p=128), self.kvf[:], "kvo", ["gb"], [], final=True)
        else:
            self.dma(self.S_f[:], self.state_hgrn[0].rearrange("h c v -> c h v"), "st0", [], SK)
            self.cp("dve", self.S_b[:], self.S_f[:], SK, SBK)
            self.dma(self.C_f[:], self.state_C[0].rearrange("h c v -> c h v"), "st1", [], ["C_f"])
            self.cp("dve", self.C_b[:], self.C_f[:], ["C_f"], ["C_b"])
            self.load_T(self.n_f[:], self.state_n[0], 4, [], ["n_f"], "st2")
            self.cp("dve", self.n_b[:], self.n_f[:], ["n_f"], ["n_b"])
            self.load_T(self.carry[:].rearrange("p f r -> p r f"), self.state_conv[0].rearrange("r (f p) -> (r f) p", p=128),
                        2 * NFC, [], ["carry"], "st2")
            self.memset("pool", self.Bx[:, 0:1], 0.0, [], ["Bx"])
            self.dma(self.Gx[:, 0:1], self.state_m[0].rearrange("(p o) -> p o", o=1), "st4", [], ["Gx"], slow=True)
            self.dma(self.kvf[:], self.cache_k[0].rearrange("(j p) d -> p j d", p=128), "st5", [], ["gb"])
            for h in range(4):
                pt, pk = self.psum()
                for mc in range(2):
                    self.tr(pt[:, mc * 128:(mc + 1) * 128], self.kvf[:, mc, h * 128:(h + 1) * 128], self.ident_f[:],
                            ["gb", "consts"], [pk])
                self.cp("act", self.KT[:, h, :], pt[:, 0:NMEM], [pk], ["KT"])
                self.pfree(pk)
            self.dma(self.kvf[:], self.cache_v[0].rearrange("(j p) d -> p j d", p=128), "st5", [], ["gb"])
            self.cp("dve", self.Vb[:], self.kvf[:], ["gb"], ["Vb"])

    def load_final_gain(self):
        self.dma(self.gb[:], self.final_norm[0].partition_broadcast(128), "gb", [], ["gb"])

    def seq_finish(self, sq):
        if sq["kind"] == "p":
            b = sq["idx"]
            oh, oc, on, om, ocv = self.o_hgrn_p[b], self.o_C_p[b], self.o_n_p[b], self.o_m_p[b], self.o_cv_p[b]
        else:
            oh, oc, on, om, ocv = self.o_hgrn_s[0], self.o_C_s[0], self.o_n_s[0], self.o_m_s[0], self.o_cv_s[0]
        TT = sq["TT"]
        SK = ["S_f%d" % h for h in range(4)]
        self.dma(oh.rearrange("h c v -> c h v"), self.S_f[:], "fo0", SK, [], final=True)
        self.dma(oc.rearrange("h c v -> c h v"), self.C_f[:], "fo1", ["C_f"], [], final=True)
        self.store_T(on, self.n_f[:], 4, ["n_f"], "fo2")
        self.dma(om.rearrange("(p o) -> p o", o=1), self.m_last[:, 0:1], "fo3", ["m_last"], [], final=True, slow=True)
        self.cp("dve", self.carryT[:].rearrange("p (r f) -> p r f", r=2), self.carry[:].rearrange("p f r -> p r f"), ["carry"], ["carryT"])
        self.store_T(ocv.rearrange("r (f p) -> (r f) p", p=128), self.carryT[:], 2 * NFC, ["carryT"], "fo2")

    def fm_group(self, w, wkey, ncol_chunks, rhs, rhs_key, TT, consume):
        KC = w.shape[1]
        for c in range(ncol_chunks):
            pt, pk = self.psum()
            for kc in range(KC):
                self.mm(pt[:, 0:TT], w[:, kc, c * 128:(c + 1) * 128], rhs[:, kc, 0:TT], kc == 0, kc == KC - 1,
                        [wkey, rhs_key], [pk])
            consume(c, pt, pk)
            self.pfree(pk)

    def tm_group(self, w, wkey, c0, P, NS, consume):
        for j in range(NS):
            pt, pk = self.psum()
            for kc in range(8):
                self.mm(pt[0:P, :], self.u_fm[:, kc, j * P:(j + 1) * P], w[:, kc, :], kc == 0, False, [wkey, "u_fm"], [pk])
            self.mm(pt[0:P, :], self.ones_bf[0:1, 0:P], self.brow_b[0:1, c0, :], False, True, ["consts"], [pk])
            consume(j, pt, pk)
            self.pfree(pk)

    def tile(self, sq, ti):
        TT, P, L = sq["TT"], sq["P"], sq["L"]
        NS = TT // P
        t0 = ti * TT
        x_ap = sq["x"][t0:t0 + TT, :].rearrange("(j p) d -> p j d", p=P)
        y_ap = sq["y"][t0:t0 + TT, :].rearrange("(j p) d -> p j d", p=P)
        xt = self.xt
        u_fm = self.u_fm
        def stage_x(j):
            xs, xk = self.tmp2()
            self.dma(xs[0:P, 0:D], x_ap[:, j, :], "xs%d" % (j % 2), [], xk)
            return xs[0:P, 0:D], xk
        self.norm_transpose(stage_x, P, NS, 0, self.u_fm, "u_fm", "provider")
        self.ck("normT")
        w, wk = self.load_piece("hq")
        self.fm_group(w, wk, 4, u_fm, "u_fm", TT, self._c_hq_factory(TT))
        w, wk = self.load_piece("hf")
        self.fm_group(w, wk, 4, u_fm, "u_fm", TT, self._c_hf_factory(TT, L))
        w, wk = self.load_piece("hi")
        self.tm_group(w, wk, 0, P, NS,
                      lambda j, pt, pk: self.cp("act", self.v_tm[0:P, j, :], pt[0:P, :], [pk], ["v_tm"]))
        w, wk = self.load_piece("hg")

        def c_hg(j, pt, pk):
            xb, xk = self.tmp()
            th, tk = self.tmp()
            self.cp("act", xb[0:P, 0:512], pt[0:P, :], [pk], [xk])
            self.act(th[0:P, 0:512], pt[0:P, :], AF.Tanh, [pk], [tk], scale=0.5)
            self.stt(xb[0:P, 0:512], th[0:P, 0:512], 1.0, xb[0:P, 0:512], ALU.add, ALU.mult, [xk, tk], [xk])
            self.tt("dve", self.gate_h[0:P, j, :], xb[0:P, 0:512], self.hgn[0:P, :], ALU.mult, [xk, "hgn"], ["gate_h"])
        self.tm_group(w, wk, 1, P, NS, c_hg)
        self.ck("hgates")
        for j in range(NS):
            self.hgrn_subtile(sq, j)
        self.ck("hgrn")
        w, wk = self.load_piece("mq")
        self.fm_group(w, wk, 4, u_fm, "u_fm", TT,
                      lambda h, pt, pk: self.act(self.mq[:, h, 0:TT], pt[:, 0:TT], AF.Identity, [pk, "bfm"], ["mq"],
                                                 bias=self.bfm_a[:, 16 + h:17 + h]))
        w, wk = self.load_piece("mk")
        self.fm_group(w, wk, 4, u_fm, "u_fm", TT,
                      lambda h, pt, pk: self.act(self.mk[:, h, 0:TT], pt[:, 0:TT], AF.Identity, [pk, "consts"], ["mk"],
                                                 bias=self.bk_s[:, h:h + 1], scale=128 ** -0.5))
        w, wk = self.load_piece("mv")
        self.tm_group(w, wk, 2, P, NS,
                      lambda j, pt, pk: self.cp("act", self.vm_tm[0:P, j, :, 0:128],
                                                pt[0:P, :].rearrange("p (h v) -> p h v", h=4), [pk], ["vm_tm"]))
        w, wk = self.load_piece("mo")

        def c_mo(j, pt, pk):
            th, tk = self.tmp()
            self.act(th[0:P, 0:512], pt[0:P, :], AF.Tanh, [pk], [tk], scale=0.5)
            self.stt(self.gate_m[0:P, j, :], th[0:P, 0:512], 1.0, self.mln[0:P, :], ALU.add, ALU.mult, [tk, "mln"], ["gate_m"])
        self.tm_group(w, wk, 3, P, NS, c_mo)
        self.ck("mproj")
        self.mlstm_rows(sq)
        self.ck("mrows")
        for j in range(NS):
            self.mlstm_subtile(sq, j)
        self.ck("mlstm")
        w, wk = self.load_piece("xq")
        self.fm_group(w, wk, 4, u_fm, "u_fm", TT,
                      lambda h, pt, pk: self.act(self.xq[:, h, 0:TT], pt[:, 0:TT], AF.Identity, [pk, "bfm"], ["xq"],
                                                 bias=self.bfm_x[:, h:h + 1]))
        for j in range(NS):
            self.attn_subtile(sq, j)
        self.ck("attn")
        srcs = [(self.o_h_fm, "o_h_fm"), (self.h_m_fm, "h_m_fm"), (self.o_x_fm, "o_x_fm")]
        for br in range(3):
            wb, wbk = self.load_piece("br%d" % br)
            src, srck = srcs[br]
            for half in range(2):
                wg, wgk = self.load_piece("gl%d" % (2 * br + half))
                for c in range(4):
                    oc = half * 4 + c
                    pg, pgk = self.psum()
                    for kc in range(8):
                        self.mm(pg[:, 0:TT], wg[:, kc, c * 128:(c + 1) * 128], u_fm[:, kc, 0:TT], kc == 0, kc == 7,
                                [wgk, "u_fm"], [pgk])
                    pp, ppk = self.psum()
                    for kc in range(4):
                        self.mm(pp[:, 0:TT], wb[:, kc, oc * 128:(oc + 1) * 128], src[:, kc, 0:TT], kc == 0, kc == 3,
                                [wbk, srck], [ppk])
                    th, tk = self.tmp()
                    gi = br * 8 + oc
                    self.act(th[:, 0:TT], pg[:, 0:TT], AF.Tanh, [pgk, "consts"], [tk], bias=self.bg_half[:, gi:gi + 1], scale=0.5)
                    self.pfree(pgk)
                    mk_ = "macc%d" % oc
                    if br == 0:
                        self.stt(self.macc[:, oc, 0:TT], th[:, 0:TT], 1.0, pp[:, 0:TT], ALU.add, ALU.mult, [tk, ppk], [mk_])
                    else:
                        self.stt(th[:, 0:TT], th[:, 0:TT], 1.0, pp[:, 0:TT], ALU.add, ALU.mult, [tk, ppk], [tk])
                        if br == 1:
                            self.tt("dve", self.macc[:, oc, 0:TT], self.macc[:, oc, 0:TT], th[:, 0:TT], ALU.add, [mk_, tk], [mk_])
                        else:
                            self.tt("dve", self.merged[:, oc, 0:TT], self.macc[:, oc, 0:TT], th[:, 0:TT], ALU.add, [mk_, tk], ["merged"])
                    self.pfree(ppk)
        self.ck("merge")
        for j in range(NS):
            self.dma(xt[0:P, j, :], x_ap[:, j, :], "xt%d" % j, [], ["xt%d" % j])
        for i in range(2):
            w, wk = self.load_piece("out%d" % i)
            for j in range(NS):
                pt, pk = self.psum()
                for kc in range(8):
                    self.mm(pt[0:P, :], self.merged[:, kc, j * P:(j + 1) * P], w[:, kc, :], kc == 0, kc == 7, [wk, "merged"], [pk])
                self.stt(xt[0:P, j, i * 512:(i + 1) * 512], pt[0:P, :], 0.5, xt[0:P, j, i * 512:(i + 1) * 512],
                         ALU.mult, ALU.add, [pk, "xt%d" % j], ["xt%d" % j])
                self.pfree(pk)
        self.norm_transpose(lambda j: xt[0:P, j, :], P, NS, 1, self.u_fm, "u_fm", None)
        self.ck("norm2")
        up_cache = {}

        def up_chunk(colchunk):
            pi, c = divmod(colchunk, 4)
            if pi not in up_cache:
                up_cache.clear()
                up_cache[pi] = self.load_piece("up%d" % pi)
            w, wk = up_cache[pi]
            pt, pk = self.psum()
            for kc in range(8):
                self.mm(pt[:, 0:TT], w[:, kc, c * 128:(c + 1) * 128], u_fm[:, kc, 0:TT], kc == 0, kc == 7, [wk, "u_fm"], [pk])
            return pt, pk
        for fc in range(NFC):
            pt, pk = up_chunk(fc)
            self.ffn_gate(sq, fc, pt, pk, TT)
            self.pfree(pk)
        for fc in range(NFC):
            pt, pk = up_chunk(NFC + fc)
            at, ak = self.act_ch[fc]
            self.tt("dve", at[:, 0:TT], at[:, 0:TT], pt[:, 0:TT], ALU.mult, [ak, pk], [ak])
            self.pfree(pk)
        self.ck("ffnup")
        for g in range(4):
            banks = [self.psum() for _ in range(NS)]
            for kh in range(2):
                w, wk = self.load_piece("dn%d_%d" % (g, kh))
                for j in range(NS):
                    pt, pk = banks[j]
                    for kc in range(11):
                        self.mm(pt[0:P, 0:256], self.act_ch[kh * 11 + kc][0][:, j * P:(j + 1) * P], w[:, kc, :],
                                kh == 0 and kc == 0, kh == 1 and kc == 10, [wk, self.act_ch[kh * 11 + kc][1]], [pk])
            for j in range(NS):
                pt, pk = banks[j]
                self.stt(xt[0:P, j, g * 256:(g + 1) * 256], pt[0:P, 0:256], 0.5, xt[0:P, j, g * 256:(g + 1) * 256],
                         ALU.mult, ALU.add, [pk, "xt%d" % j], ["xt%d" % j])
                self.pfree(pk)
        self.ck("ffndn")
        for j in range(NS):
            src = xt[0:P, j, :]
            xk = "xt%d" % j
            rs, sk = self.rstd_rows(src, P, [xk], 2)
            self.stt(src, src, rs, self.gb[0:P, :], ALU.mult, ALU.mult, [xk, sk, "gb"], [xk])
            self.dma(y_ap[:, j, :], src, "yo%d" % j, [xk], [], final=True)

    def _c_hq_factory(self, TT):
        def c_hq(h, pt, pk):
            xb, xk = self.tmp()
            th, tk = self.tmp()
            self.act(xb[:, 0:TT], pt[:, 0:TT], AF.Identity, [pk, "bfm"], [xk], bias=self.bfm_a[:, h:h + 1])
            self.act(th[:, 0:TT], pt[:, 0:TT], AF.Tanh, [pk, "consts"], [tk], bias=self.bq_half[:, h:h + 1], scale=0.5)
            self.stt(self.qt[:, h, 0:TT], th[:, 0:TT], 1.0, xb[:, 0:TT], ALU.add, ALU.mult, [xk, tk], ["qt"])
        return c_hq

    def _c_hf_factory(self, TT, L):
        def c_hf(h, pt, pk):
            e_, ek = self.tmp()
            l1, l1k = self.tmp()
            A_, Ak = self.tmp()
            en, enk = self.tmp()
            l2, l2k = e_, ek
            f_, fk = e_, ek
            self.act(e_[:, 0:TT], pt[:, 0:TT], AF.Exp, [pk, "consts"], [ek], bias=self.bf_neg[:, h:h + 1], scale=-1.0)
            self.act(l1[:, 0:TT], e_[:, 0:TT], AF.Ln, [ek, "lb"], [l1k], bias=self.one_col[:, 0:1], scale=self.lb[:, h:h + 1])
            self.act(l2[:, 0:TT], e_[:, 0:TT], AF.Ln, [ek], [l2k], bias=self.one_col[:, 0:1])
            self.tt("dve", l1[:, 0:TT], l1[:, 0:TT], l2[:, 0:TT], ALU.subtract, [l1k, l2k], [l1k])
            self.S.op("dve", lambda e: e.tensor_tensor_scan(out=A_[:, 0:TT], data0=self.rmask[:, 0:TT], data1=l1[:, 0:TT],
                                                            initial=0.0, op0=ALU.mult, op1=ALU.add), [l1k, "consts"], [Ak])
            self.act(f_[:, 0:TT], l1[:, 0:TT], AF.Exp, [l1k], [fk])
            self.ts("dve", f_[:, 0:TT], f_[:, 0:TT], -1.0, 1.0, ALU.mult, ALU.add, [fk], [fk])
            self.act(en[:, 0:TT], A_[:, 0:TT], AF.Exp, [Ak], [enk], scale=-1.0)
            self.act(self.EA[:, h, 0:TT], A_[:, 0:TT], AF.Exp, [Ak], ["EA"])
            nch = TT // L
            self.act(self.dec[:, h, 0:nch].rearrange("p (c o) -> p c o", o=1),
                     A_[:, 0:TT].rearrange("p (c l) -> p c l", l=L)[:, :, L - 1:L], AF.Exp, [Ak], ["dec"])
            self.tt("dve", self.kt[:, h, 0:TT], f_[:, 0:TT], en[:, 0:TT], ALU.mult, [fk, enk], ["kt"])
            self.stt(self.qt[:, h, 0:TT], self.qt[:, h, 0:TT], 0.5, self.EA[:, h, 0:TT], ALU.mult, ALU.mult, ["qt", "EA"], ["qt"])
        return c_hf

    def hgrn_subtile(self, sq, j):
        P, L = sq["P"], sq["L"]
        NCH = P // L
        c0 = j * P
        pt, pk = self.psum()
        ptb = pt[:].bitcast(BF16)
        for h in range(4):
            for c in range(NCH):
                cg = (c0 + c * L) // L
                self.ts("pool", self.kp[:, h, c * L:(c + 1) * L], self.kt[:, h, c0 + c * L:c0 + (c + 1) * L],
                        self.dec[:, h, cg:cg + 1], None, ALU.mult, None, ["kt", "dec"], ["kp"])
        for h in range(4):
            self.tr(ptb[0:P, h * 128:(h + 1) * 128], self.kp[:, h, 0:P], self.ident_b[:], ["kp", "consts"], [pk])
        self.cp("act", self.kpT[0:P, :], ptb[0:P, 0:512], [pk], ["kpT"])
        self.pfree(pk)
        po, pok = self.psum()
        for h in range(4):
            ps_, psk = self.psum()
            self.mm(ps_[0:P, 0:P], self.kt[:, h, c0:c0 + P], self.qt[:, h, c0:c0 + P], True, True, ["kt", "qt"], [psk])
            self.tt("dve", self.STh[0:P, 0:P], ps_[0:P, 0:P], self.maskH[0:P, 0:P], ALU.mult, [psk, "consts"], ["STh"])
            self.pfree(psk)
            sbk = "S_b%d" % h
            sfk = "S_f%d" % h
            snaps = []
            for c in range(NCH):
                r0 = c * L
                snap = self.Ssnap[c][:, h, :]
                snk = "Ssnap%d_%d" % (c, h)
                self.cp("pool", snap, self.S_b[:, h, :], [sbk], [snk])
                snaps.append((snap, snk))
                pu, puk = self.psum()
                self.mm(pu[:, 0:128], self.kpT[r0:r0 + L, h * 128:(h + 1) * 128], self.v_tm[r0:r0 + L, j, h * 128:(h + 1) * 128],
                        True, True, ["kpT", "v_tm"], [puk])
                cg = (c0 + c * L) // L
                self.stt(self.S_f[:, h, :], self.S_f[:, h, :], self.dec[:, h, cg:cg + 1], pu[:, 0:128], ALU.mult, ALU.add,
                         [sfk, "dec", puk], [sfk])
                self.pfree(puk)
                self.cp("act", self.S_b[:, h, :], self.S_f[:, h, :], [sfk], [sbk])
            self.mm(po[0:P, h * 128:(h + 1) * 128], self.STh[0:P, 0:P], self.v_tm[0:P, j, h * 128:(h + 1) * 128], True, False,
                    ["STh", "v_tm"], [pok])
            for c in range(NCH):
                r0 = c * L
                snap, snk = snaps[c]
                self.mm(po[r0:r0 + L, h * 128:(h + 1) * 128], self.qt[:, h, c0 + r0:c0 + r0 + L], snap, False, True,
                        ["qt", snk], [pok])
        for h in range(4):
            jt, jk = self.tmp()
            self.act(jt[0:P, 0:128], po[0:P, h * 128:(h + 1) * 128], AF.Square, [pok], ["ssq_h", jk],
                     accum=self.ssq_h[0:P, 4 + h:5 + h])
        self.ts("dve", self.ssq_h[0:P, 8:12], self.ssq_h[0:P, 4:8], 1.0 / 128, EPS, ALU.mult, ALU.add, ["ssq_h"], ["ssq_h"])
        self.pow_m05(self.ssq_h[0:P, 12:16], self.ssq_h[0:P, 8:12], ["ssq_h"], ["ssq_h"])
        for h in range(4):
            self.stt(self.oh_tm[0:P, h * 128:(h + 1) * 128], po[0:P, h * 128:(h + 1) * 128], self.ssq_h[0:P, 12 + h:13 + h],
                     self.gate_h[0:P, j, h * 128:(h + 1) * 128], ALU.mult, ALU.mult, [pok, "ssq_h", "gate_h"], ["oh_tm"])
        self.pfree(pok)
        self.tm_to_fm(self.oh_tm, "oh_tm", P, self.o_h_fm, "o_h_fm", c0)

    def tm_to_fm(self, src, srck, P, dst, dstk, c0):
        pt, pk = self.psum()
        ptb = pt[:].bitcast(BF16)
        for h in range(4):
            self.tr(ptb[:, h * P:(h + 1) * P], src[0:P, h * 128:(h + 1) * 128], self.ident_b[0:P, 0:P], [srck, "consts"], [pk])
        self.cp("act", dst[:, :, c0:c0 + P], ptb[:, 0:4 * P].rearrange("p (h t) -> p h t", h=4), [pk], [dstk])
        self.pfree(pk)

    def mlstm_rows(self, sq):
        TT = sq["TT"]
        R = self.rows
        sp, Dr, a_, mt = (R[:, i, 0:TT] for i in range(4))
        nG, et = sp, mt
        K = ["rows"]
        pi, pik = self.psum()
        for kc in range(8):
            self.mm(pi[0:4, 0:TT], self.w_if[:, kc, 0:4], self.u_fm[:, kc, 0:TT], kc == 0, kc == 7, ["w_if", "u_fm"], [pik])
        pf, pfk = self.psum()
        for kc in range(8):
            self.mm(pf[0:4, 0:TT], self.w_if[:, kc, 4:8], self.u_fm[:, kc, 0:TT], kc == 0, kc == 7, ["w_if", "u_fm"], [pfk])
        if self._rows_started:
            pT = self._prevTT
            self.cp("dve", self.Bx[:, 0:1], self.Bx[:, pT:pT + 1], ["Bx"], ["Bx"])
            self.cp("dve", self.Gx[:, 0:1], self.Gx[:, pT:pT + 1], ["Gx"], ["Gx"])
        self._rows_started = True
        self._prevTT = TT
        self.act(sp, pf[0:4, 0:TT], AF.Exp, [pfk, "bfn"], K, bias=self.bfn[:, 0:1], scale=-1.0)
        self.pfree(pfk)
        self.act(sp, sp, AF.Ln, K, K, bias=self.one_col[0:4, 0:1])
        self.S.op("dve", lambda e: e.tensor_tensor_scan(out=self.Bx[:, 1:TT + 1], data0=self.ones4[:, 0:TT], data1=sp,
                                                        initial=self.Bx[:, 0:1], op0=ALU.mult, op1=ALU.subtract),
                  K + ["Bx", "consts"], ["Bx"])
        self.stt(Dr, pi[0:4, 0:TT], self.bi[:, 0:1], self.Bx[:, 1:TT + 1], ALU.add, ALU.subtract, [pik, "bi", "Bx"] + K, K)
        self.pfree(pik)
        self.S.op("dve", lambda e: e.tensor_tensor_scan(out=self.Gx[:, 1:TT + 1], data0=self.ones4[:, 0:TT], data1=Dr,
                                                        initial=self.Gx[:, 0:1], op0=ALU.mult, op1=ALU.max),
                  K + ["Gx", "consts"], ["Gx"])
        self.ts("dve", nG, self.Gx[:, 1:TT + 1], -1.0, None, ALU.mult, None, ["Gx"] + K, K)
        self.tt("dve", mt, self.Bx[:, 1:TT + 1], self.Gx[:, 1:TT + 1], ALU.add, ["Bx", "Gx"] + K, K)
        self.cp("dve", self.m_last[:, 0:1], R[:, 3, TT - 1:TT], K, ["m_last"])
        self.act(et, mt, AF.Exp, K, K, scale=-1.0)

    def mlstm_subtile(self, sq, j):
        P = sq["P"]
        c0 = j * P
        R = self.rows
        K = ["rows"]
        self.act(R[:, 2, c0:c0 + P], R[:, 0, c0:c0 + P], AF.Exp, K + ["Gx"], K, bias=self.Gx[:, c0:c0 + 1])
        pt, pk = self.psum()
        for q, ri in enumerate((1, 2, 3)):
            self.tr(pt[0:P, 4 * q:4 * q + 4], R[:, ri, c0:c0 + P], self.ident_f[0:4, 0:4], K + ["consts"], [pk])
        self.cp("dve", self.sc_tm[0:P, 0:12], pt[0:P, 0:12], [pk], ["sc_tm"])
        self.pfree(pk)
        pd, pdk = self.psum()
        for h in range(4):
            self.mm(pd[:, h:h + 1], self.sel[:, h, :], R[:, 2, c0 + P - 1:c0 + P], True, True, K + ["consts"], [pdk])
        self.cp("dve", self.decm[:, :], pd[:, 0:4], [pdk], ["decm"])
        self.pfree(pdk)
        pg, pgk = self.psum()
        for h in range(4):
            self.mm(pg[0:P, h * 128:h * 128 + P], self.sel[:, h, 0:P], R[:, 0, c0:c0 + P], True, False, K + ["consts"], [pgk])
            self.mm(pg[0:P, h * 128:h * 128 + P], self.ident_f[0:P, 0:P], self.maskM[0:P, 0:P], False, True, ["consts"], [pgk])
        for h in range(4):
            self.act(self.Wm[0:P, h, 0:P], pg[0:P, h * 128:h * 128 + P], AF.Exp, [pgk, "sc_tm"], ["Wm"], bias=self.sc_tm[0:P, h:h + 1])
        self.pfree(pgk)
        pq, pqk = self.psum()
        for h in range(4):
            self.mm(pq[0:P, h * 128:h * 128 + P], self.mk[:, h, c0:c0 + P], self.mq[:, h, c0:c0 + P], True, True, ["mk", "mq"], [pqk])
        for h in range(4):
            self.tt("dve", self.STm[0:P, h, 0:P], pq[0:P, h * 128:h * 128 + P], self.Wm[0:P, h, 0:P], ALU.mult, [pqk, "Wm"], ["STm"])
        self.pfree(pqk)
        pk2, pk2k = self.psum()
        pk2b = pk2[:].bitcast(BF16)
        for h in range(4):
            self.tr(pk2b[0:P, h * 128:(h + 1) * 128], self.mk[:, h, c0:c0 + P], self.ident_b[:], ["mk", "consts"], [pk2k])
        for h in range(4):
            self.ts("dve", self.kw_tm[0:P, h, :], pk2b[0:P, h * 128:(h + 1) * 128], self.Wm[0:P, h, P - 1:P], None, ALU.mult, None,
                    [pk2k, "Wm"], ["kw_tm"])
        self.pfree(pk2k)
        pa, pak = self.psum()
        pb, pbk = self.psum()
        pdn, pdnk = self.psum()
        for h in range(4):
            self.mm(pa[0:P, h * 128:(h + 1) * 128], self.STm[0:P, h, 0:P], self.vm_tm[0:P, j, h, 0:128], True, True, ["STm", "vm_tm"], [pak])
        for h in range(4):
            self.mm(pb[0:P, h * 128:(h + 1) * 128], self.mq[:, h, c0:c0 + P], self.C_b[:, h, :], True, True, ["mq", "C_b"], [pbk])
        for h in range(4):
            self.mm(pdn[0:P, h:h + 1], self.STm[0:P, h, 0:P], self.vm_tm[0:P, j, h, 128:129], True, True, ["STm", "vm_tm"], [pdnk])
        for h in range(4):
            self.mm(pdn[0:P, 4 + h:5 + h], self.mq[:, h, c0:c0 + P], self.n_b[:, h:h + 1], True, True, ["mq", "n_b"], [pdnk])
        for h in range(4):
            self.act(self.numI[0:P, h * 128:(h + 1) * 128], pb[0:P, h * 128:(h + 1) * 128], AF.Identity, [pbk, "sc_tm"], ["numI"],
                     scale=self.sc_tm[0:P, 4 + h:5 + h])
        self.pfree(pbk)
        self.tt("dve", self.num[0:P, :], pa[0:P, :], self.numI[0:P, :], ALU.add, [pak, "numI"], ["num"])
        self.pfree(pak)
        ds = self.dsm
        Kd = ["dsm"]
        self.cp("dve", ds[0:P, 0:8], pdn[0:P, 0:8], [pdnk] + Kd, Kd)
        self.pfree(pdnk)
        self.tt("dve", ds[0:P, 8:12], ds[0:P, 4:8], self.sc_tm[0:P, 4:8], ALU.mult, Kd + ["sc_tm"], Kd)
        self.tt("dve", ds[0:P, 8:12], ds[0:P, 8:12], ds[0:P, 0:4], ALU.add, Kd, Kd)
        self.ts("dve", ds[0:P, 12:16], ds[0:P, 8:12], -1.0, None, ALU.mult, None, Kd, Kd)
        self.tt("dve", ds[0:P, 12:16], ds[0:P, 12:16], ds[0:P, 8:12], ALU.max, Kd, Kd)
        self.tt("dve", ds[0:P, 12:16], ds[0:P, 12:16], self.sc_tm[0:P, 8:12], ALU.max, Kd + ["sc_tm"], Kd)
        self.S.op("dve", lambda e: e.reciprocal(out=ds[0:P, 16:20], in_=ds[0:P, 12:16]), Kd, Kd)
        for h in range(4):
            jt, jk = self.tmp()
            self.act(jt[0:P, 0:128], self.num[0:P, h * 128:(h + 1) * 128], AF.Square, ["num"], ["ssq_m", jk],
                     accum=self.ssq_m[0:P, h:h + 1])
        self.tt("dve", ds[0:P, 20:24], ds[0:P, 16:20], ds[0:P, 16:20], ALU.mult, Kd, Kd)
        self.tt("dve", ds[0:P, 20:24], ds[0:P, 20:24], self.ssq_m[0:P, 0:4], ALU.mult, Kd + ["ssq_m"], Kd)
        self.ts("dve", ds[0:P, 20:24], ds[0:P, 20:24], 1.0 / 128, EPS, ALU.mult, ALU.add, Kd, Kd)
        self.pow_m05(ds[0:P, 24:28], ds[0:P, 20:24], Kd, Kd)
        self.tt("dve", ds[0:P, 28:32], ds[0:P, 24:28], ds[0:P, 16:20], ALU.mult, Kd, Kd)
        for h in range(4):
            self.stt(self.hm_tm[0:P, h * 128:(h + 1) * 128], self.num[0:P, h * 128:(h + 1) * 128], ds[0:P, 28 + h:29 + h],
                     self.gate_m[0:P, j, h * 128:(h + 1) * 128], ALU.mult, ALU.mult, ["num", "dsm", "gate_m"], ["hm_tm"])
        self.tm_to_fm(self.hm_tm, "hm_tm", P, self.h_m_fm, "h_m_fm", c0)
        pus = []
        for h in range(4):
            if h % 2 == 0:
                pu, puk = self.psum()
            off = (h % 2) * 256
            self.mm(pu[:, off:off + 129], self.kw_tm[0:P, h, :], self.vm_tm[0:P, j, h, 0:129], True, True, ["kw_tm", "vm_tm"], [puk])
            pus.append((pu, puk, off))
        for h in range(4):
            pu, puk, off = pus[h]
            self.stt(self.C_f[:, h, :], self.C_f[:, h, :], self.decm[:, h:h + 1], pu[:, off:off + 128], ALU.mult, ALU.add,
                     ["C_f", "decm", puk], ["C_f"])
            self.stt(self.n_f[:, h:h + 1], self.n_f[:, h:h + 1], self.decm[:, h:h + 1], pu[:, off + 128:off + 129], ALU.mult, ALU.add,
                     ["n_f", "decm", puk], ["n_f"])
            if h % 2 == 1:
                self.pfree(puk)
        self.cp("act", self.C_b[:], self.C_f[:], ["C_f"], ["C_b"])
        self.cp("act", self.n_b[:], self.n_f[:], ["n_f"], ["n_b"])

    def attn_subtile(self, sq, j):
        P = sq["P"]
        c0 = j * P
        sc = 128 ** -0.5
        banks = [self.psum(), self.psum()]
        for h in range(4):
            pl, plk = banks[h // 2]
            off = (h % 2) * NMEM
            self.mm(pl[0:P, off:off + NMEM], self.xq[:, h, c0:c0 + P], self.KT[:, h, :], True, True, ["xq", "KT"], [plk])
        ds = self.dsa
        for hh in range(2):
            pl, plk = banks[hh]
            self.S.op("dve", lambda e, pl=pl, hh=hh: e.tensor_reduce(out=ds[0:P, 2 * hh:2 * hh + 2],
                                                                     in_=pl[0:P, :].rearrange("p (h m) -> p h m", h=2),
                                                                     axis=AX.X, op=ALU.max), [plk, "dsa"], ["dsa"])
        self.ts("dve", ds[0:P, 4:8], ds[0:P, 0:4], -sc, None, ALU.mult, None, ["dsa"], ["dsa"])
        for h in range(4):
            pl, plk = banks[h // 2]
            off = (h % 2) * NMEM
            self.act(self.pex[0:P, h, :], pl[0:P, off:off + NMEM], AF.Exp, [plk, "dsa"], ["pex", "ssq_a"], bias=ds[0:P, 4 + h:5 + h], scale=sc,
                     accum=self.ssq_a[0:P, h:h + 1])
        self.pfree(banks[0][1])
        self.pfree(banks[1][1])
        self.S.op("dve", lambda e: e.reciprocal(out=ds[0:P, 8:12], in_=self.ssq_a[0:P, 0:4]), ["ssq_a", "dsa"], ["dsa"])
        for h in range(4):
            self.ts("dve", self.pex[0:P, h, :], self.pex[0:P, h, :], ds[0:P, 8 + h:9 + h], None, ALU.mult, None, ["pex", "dsa"], ["pex"])
        pt, pk = self.psum()
        ptb = pt[:].bitcast(BF16)
        for h in range(4):
            for mc in range(2):
                q = h * 2 + mc
                self.tr(ptb[:, q * P:(q + 1) * P], self.pex[0:P, h, mc * 128:(mc + 1) * 128], self.ident_b[0:P, 0:P], ["pex", "consts"], [pk])
        self.cp("act", self.pT[:, :, 0:P], ptb[:, 0:8 * P].rearrange("p (q t) -> p q t", q=8), [pk], ["pT"])
        self.pfree(pk)
        po, pok = self.psum()
        for h in range(4):
            for mc in range(2):
                self.mm(po[:, h * P:(h + 1) * P], self.Vb[:, mc, h * 128:(h + 1) * 128], self.pT[:, h * 2 + mc, 0:P], mc == 0, mc == 1,
                        ["Vb", "pT"], [pok])
        self.cp("dve", self.o_x_fm[:, :, c0:c0 + P], po[:, 0:4 * P].rearrange("p (h t) -> p h t", h=4), [pok], ["o_x_fm"])
        self.pfree(pok)

    def ffn_gate(self, sq, fc, pt, pk, TT):
        gx = self.Gxb[self.gxi]
        gk = "Gxb%d" % self.gxi
        self.gxi = (self.gxi + 1) % 2
        ck = "carry"
        self.cp("act", gx[:, 2:TT + 2], pt[:, 0:TT], [pk], [gk])
        self.cp("pool", gx[:, 0:2], self.carry[:, fc, :], [ck, gk], [gk])
        self.cp("pool", self.carry[:, fc, :], gx[:, TT:TT + 2], [gk, ck], [ck])
        cv, cvk = self.tmp()
        x2, x2k = self.tmp()
        self.act(cv[:, 0:TT], pt[:, 0:TT], AF.Identity, [pk, "cw"], [cvk], bias=self.cb[:, fc:fc + 1], scale=self.cw[:, 2, fc:fc + 1])
        self.stt(cv[:, 0:TT], gx[:, 1:TT + 1], self.cw[:, 1, fc:fc + 1], cv[:, 0:TT], ALU.mult, ALU.add, [gk, "cw", cvk], [cvk])
        self.stt(cv[:, 0:TT], gx[:, 0:TT], self.cw[:, 0, fc:fc + 1], cv[:, 0:TT], ALU.mult, ALU.add, [gk, "cw", cvk], [cvk])
        self.act(x2[:, 0:TT], cv[:, 0:TT], AF.Square, [cvk], [x2k])
        self.ts("pool", x2[:, 0:TT], x2[:, 0:TT], 0.044715, 1.0, ALU.mult, ALU.add, [x2k], [x2k])
        self.tt("pool", x2[:, 0:TT], x2[:, 0:TT], cv[:, 0:TT], ALU.mult, [x2k, cvk], [x2k])
        self.act(x2[:, 0:TT], x2[:, 0:TT], AF.Tanh, [x2k], [x2k], scale=0.7978845608028654)
        at, ak = self.act_ch[fc]
        self.stt(at[:, 0:TT], x2[:, 0:TT], 1.0, cv[:, 0:TT], ALU.add, ALU.mult, [x2k, cvk], [ak])


_CACHE = {}


def _get_nc():
    if "nc" not in _CACHE:
        b = Builder(TT=512)
        _CACHE["b"] = b
        _CACHE["nc"] = b.build()
    return _CACHE["nc"]


def kernel(**inputs):
    n = 8
    nc = _get_nc()
    f = lambda a: np.ascontiguousarray(np.asarray(a, dtype=np.float32))
    in_maps = []
    for c in range(n):
        m = {
            "x_prompt": f(inputs["x_prompt"][2 * c:2 * c + 2]),
            "x_sample": f(inputs["x_sample"][c:c + 1]),
            "state_hgrn": f(inputs["state_hgrn"][0, c:c + 1]),
            "state_mlstm_C": f(inputs["state_mlstm_C"][0, c:c + 1]),
            "state_mlstm_n": f(inputs["state_mlstm_n"][0, c:c + 1]),
            "state_mlstm_m": f(inputs["state_mlstm_m"][0, c:c + 1]),
            "state_ffn_conv": f(inputs["state_ffn_conv"][0, c:c + 1]),
            "cache_mem_k": f(inputs["cache_mem_k"][0, c:c + 1]).reshape(1, NMEM, 512),
            "cache_mem_v": f(inputs["cache_mem_v"][0, c:c + 1]).reshape(1, NMEM, 512),
            "mem_prompt": f(inputs["mem_prompt"][2 * c:2 * c + 2]),
            "norm1": f(inputs["norm1"]),
            "w_in": f(inputs["w_in"][0]),
            "b_in": f(inputs["b_in"]),
            "ml_fgate_bias": f(inputs["ml_fgate_bias"]),
            "hg_lb_logits": f(inputs["hg_lb_logits"]),
            "hg_norm": f(inputs["hg_norm"]),
            "ml_norm": f(inputs["ml_norm"]),
            "mem_norm": f(inputs["mem_norm"]),
            "w_mem_kv": f(inputs["w_mem_kv"][0]),
            "w_branch": f(inputs["w_branch"][0]),
            "w_out": f(inputs["w_out"][0]),
            "norm2": f(inputs["norm2"]),
            "w_up": f(inputs["w_up"][0]),
            "ffn_conv_w": f(inputs["ffn_conv_w"][0]),
            "ffn_conv_b": f(inputs["ffn_conv_b"]),
            "w_down": f(inputs["w_down"][0]),
            "final_norm": f(inputs["final_norm"]).reshape(1, D),
        }
        in_maps.append(m)
    res = run_bass_kernel_spmd(nc, in_maps, core_ids=list(range(n)))
    R = res.results
    cat = lambda k: np.concatenate([np.asarray(r[k]) for r in R], axis=0)
    outs = (
        cat("y_prompt"), cat("y_sample"),
        cat("hgrn_p")[None], cat("mlstm_C_p")[None], cat("mlstm_n_p")[None], cat("mlstm_m_p")[None],
        cat("mem_k_p").reshape(1, 16, NMEM, 4, 128), cat("mem_v_p").reshape(1, 16, NMEM, 4, 128),
        cat("ffn_conv_p")[None],
        cat("hgrn_s")[None], cat("mlstm_C_s")[None], cat("mlstm_n_s")[None], cat("mlstm_m_s")[None],
        cat("ffn_conv_s")[None],
    )
    return tuple(np.ascontiguousarray(o, dtype=np.float32) for o in outs)
```
